# Optimizing a Trainium2 kernel written in Bass

```python
import math
import jax
import jax.numpy as jnp
from jax import lax
import numpy as np

D_MODEL = 1024
BATCH = 16
SEQ = 2048
DEPTH = 1

SSM_EXPAND = 2
D_INNER = SSM_EXPAND * D_MODEL
SSM_HEAD_DIM = 64
SSM_HEADS = D_INNER // SSM_HEAD_DIM
SSM_GROUPS = 4
D_STATE = 128
CONV_WIDTH = 4
SSD_CHUNK = 128
CONV_DIM = D_INNER + 2 * SSM_GROUPS * D_STATE
ATTN_HEADS = 16
KV_HEADS = 4
HEAD_DIM = 64
WINDOW = 128
N_EXPERTS = 32
TOP_K = 4
D_FF = D_MODEL
SWIGLU_LIMIT = 7.0
SWIGLU_ALPHA = 1.702
MOE_BLOCK = 256
N_BRANCHES = 2
LN_EPS = 1e-5
RMS_EPS = 1e-5
DEEPNORM_ALPHA = (2 * DEPTH) ** 0.25
DEEPNORM_BETA = (8 * DEPTH) ** -0.25
IN_SPLITS = (D_INNER, CONV_DIM, SSM_HEADS, ATTN_HEADS * HEAD_DIM, KV_HEADS * HEAD_DIM, KV_HEADS * HEAD_DIM, N_BRANCHES * D_MODEL)
IN_DIM = sum(IN_SPLITS)
V_SPLIT_INDEX = 5

kernel_name = 'hybrid_ssd_swa_moe_deepnorm'


def _split_points():
    return [int(o) for o in np.cumsum(IN_SPLITS)[:-1]]


def layer_norm(x, g, b):
    xf = x.astype(jnp.float32)
    mu = jnp.mean(xf, axis=-1, keepdims=True)
    var = jnp.mean(jnp.square(xf - mu), axis=-1, keepdims=True)
    return ((xf - mu) * lax.rsqrt(var + LN_EPS) * g.astype(jnp.float32) + b.astype(jnp.float32)).astype(x.dtype)


def causal_depthwise_conv(u, w, bias):
    out = lax.conv_general_dilated(
        u, w[:, None, :].astype(u.dtype), window_strides=(1,), padding=[(CONV_WIDTH - 1, 0)],
        dimension_numbers=('NWC', 'WIO', 'NWC'), feature_group_count=u.shape[-1])
    return out + bias.astype(u.dtype)


def ssd_chunked_scan(xh, dt, A, Bm, Cm):
    b, s, h, p = xh.shape
    g, n = Bm.shape[2], Bm.shape[3]
    e = h // g
    l = SSD_CHUNK
    c = s // l
    xs = (xh.astype(jnp.float32) * dt[..., None]).reshape(b, c, l, g, e, p)
    Bc = Bm.astype(jnp.float32).reshape(b, c, l, g, n)
    Cc = Cm.astype(jnp.float32).reshape(b, c, l, g, n)
    a_cum = jnp.cumsum((dt * A).reshape(b, c, l, g, e), axis=2)
    seg = a_cum[:, :, :, None] - a_cum[:, :, None, :]
    causal = jnp.tril(jnp.ones((l, l), dtype=bool))[None, None, :, :, None, None]
    decay = jnp.exp(jnp.where(causal, seg, -jnp.inf))
    cb = jnp.einsum('bctgn,bcsgn->bctsg', Cc, Bc)
    y_diag = jnp.einsum('bctsg,bctsge,bcsgep->bctgep', cb, decay, xs)
    decay_to_end = jnp.exp(a_cum[:, :, -1:] - a_cum)
    states = jnp.einsum('bclgn,bclge,bclgep->bcgepn', Bc, decay_to_end, xs)
    chunk_decay = jnp.exp(a_cum[:, :, -1])

    def step(carry, inp):
        st, dec = inp
        return carry * dec[..., None, None] + st, carry

    init = jnp.zeros((b, g, e, p, n), jnp.float32)
    _, prev = lax.scan(step, init, (jnp.moveaxis(states, 1, 0), jnp.moveaxis(chunk_decay, 1, 0)))
    prev = jnp.moveaxis(prev, 0, 1)
    y_off = jnp.einsum('bctgn,bcgepn,bctge->bctgep', Cc, prev, jnp.exp(a_cum))
    return (y_diag + y_off).reshape(b, s, h, p)


def mamba2_branch(z, xbc, dt_raw, conv_w, conv_b, dt_bias, a_log, d_skip, norm_w):
    b, s, _ = z.shape
    xbc = jax.nn.silu(causal_depthwise_conv(xbc, conv_w, conv_b))
    xh, Bm, Cm = jnp.split(xbc, [D_INNER, D_INNER + SSM_GROUPS * D_STATE], axis=-1)
    xh = xh.reshape(b, s, SSM_HEADS, SSM_HEAD_DIM)
    Bm = Bm.reshape(b, s, SSM_GROUPS, D_STATE)
    Cm = Cm.reshape(b, s, SSM_GROUPS, D_STATE)
    dt = jax.nn.softplus(dt_raw.astype(jnp.float32) + dt_bias.astype(jnp.float32))
    A = -jnp.exp(a_log.astype(jnp.float32))
    y = ssd_chunked_scan(xh, dt, A, Bm, Cm)
    y = y + xh.astype(jnp.float32) * d_skip.astype(jnp.float32)[:, None]
    y = y.reshape(b, s, D_INNER) * jax.nn.silu(z.astype(jnp.float32))
    yg = y.reshape(b, s, SSM_GROUPS, D_INNER // SSM_GROUPS)
    yg = yg * lax.rsqrt(jnp.mean(yg * yg, axis=-1, keepdims=True) + RMS_EPS)
    return (yg.reshape(b, s, D_INNER) * norm_w.astype(jnp.float32)).astype(z.dtype)


def sliding_window_attention(q, k, v, sinks):
    b, s, _ = q.shape
    nb = s // WINDOW
    grp = ATTN_HEADS // KV_HEADS
    qb = q.reshape(b, nb, WINDOW, KV_HEADS, grp, HEAD_DIM)
    kb = k.reshape(b, nb, WINDOW, KV_HEADS, HEAD_DIM)
    vb = v.reshape(b, nb, WINDOW, KV_HEADS, HEAD_DIM)

    def with_prev(t):
        prev = jnp.pad(t, ((0, 0), (1, 0), (0, 0), (0, 0), (0, 0)))[:, :-1]
        return jnp.concatenate([prev, t], axis=2)

    kk, vv = with_prev(kb), with_prev(vb)
    scores = jnp.einsum('bnqkgd,bnskd->bnkgqs', qb, kk).astype(jnp.float32) * (HEAD_DIM ** -0.5)
    qpos = jnp.arange(WINDOW)[:, None] + WINDOW
    kpos = jnp.arange(2 * WINDOW)[None, :]
    band = (qpos - kpos >= 0) & (qpos - kpos < WINDOW)
    has_prev = (jnp.arange(nb)[:, None] > 0) | (kpos >= WINDOW)
    mask = band[None] & has_prev[:, None, :]
    scores = jnp.where(mask[None, :, None, None], scores, -jnp.inf)
    sink = jnp.broadcast_to(sinks.astype(jnp.float32).reshape(1, 1, KV_HEADS, grp, 1, 1), scores.shape[:-1] + (1,))
    probs = jax.nn.softmax(jnp.concatenate([scores, sink], axis=-1), axis=-1)[..., :-1]
    out = jnp.einsum('bnkgqs,bnskd->bnqkgd', probs.astype(v.dtype), vv)
    return out.reshape(b, s, ATTN_HEADS * HEAD_DIM)


def token_mixer(x, w_in, conv_w, conv_b, dt_bias, a_log, d_skip, ssm_norm_w, w_ssm_out,
                attn_sinks, w_attn_out, b_gates, w_mix_out):
    b, s, _ = x.shape
    proj = x @ w_in
    z, xbc, dt_raw, q, k, v, gate_logits = jnp.split(proj, _split_points(), axis=-1)
    y_ssm = mamba2_branch(z, xbc, dt_raw, conv_w, conv_b, dt_bias, a_log, d_skip, ssm_norm_w) @ w_ssm_out
    y_attn = sliding_window_attention(q, k, v, attn_sinks) @ w_attn_out
    gates = jax.nn.sigmoid((gate_logits + b_gates).astype(jnp.float32)).reshape(b, s, N_BRANCHES, D_MODEL)
    merged = gates[:, :, 0] * y_ssm.astype(jnp.float32) + gates[:, :, 1] * y_attn.astype(jnp.float32)
    return merged.astype(x.dtype) @ w_mix_out


def moe_ffn(h, w_router, b_router, w_gate_up, b_gate_up, w_down, b_down):
    b, s, d = h.shape
    n_tok = b * s
    n_slots = n_tok * TOP_K
    xt = h.reshape(n_tok, d)
    logits = xt.astype(jnp.float32) @ w_router.astype(jnp.float32) + b_router.astype(jnp.float32)
    top_logits, top_idx = lax.top_k(logits, TOP_K)
    gate_w = jax.nn.softmax(top_logits, axis=-1)
    flat_e = top_idx.reshape(-1)
    order = jnp.argsort(flat_e)
    sorted_e = flat_e[order]
    counts = jnp.bincount(flat_e, length=N_EXPERTS)
    padded = (counts + MOE_BLOCK - 1) // MOE_BLOCK * MOE_BLOCK
    pad_end = jnp.cumsum(padded)
    pad_start = pad_end - padded
    start = jnp.cumsum(counts) - counts
    dest = pad_start[sorted_e] + (jnp.arange(n_slots) - start[sorted_e])
    n_blocks = -(-n_slots // MOE_BLOCK) + N_EXPERTS
    n_rows = n_blocks * MOE_BLOCK
    row_tok = jnp.zeros((n_rows,), jnp.int32).at[dest].set((order // TOP_K).astype(jnp.int32))
    block_expert = jnp.minimum(jnp.searchsorted(pad_end, jnp.arange(n_blocks) * MOE_BLOCK, side='right'), N_EXPERTS - 1)
    xs = xt[row_tok].reshape(n_blocks, MOE_BLOCK, d)

    def expert_block(args):
        xb, e = args
        hgu = xb @ w_gate_up[e] + b_gate_up[e]
        glu, lin = jnp.split(hgu, 2, axis=-1)
        glu = jnp.minimum(glu, SWIGLU_LIMIT)
        lin = jnp.clip(lin, -SWIGLU_LIMIT, SWIGLU_LIMIT)
        act = glu * jax.nn.sigmoid(SWIGLU_ALPHA * glu) * (lin + 1.0)
        return act @ w_down[e] + b_down[e]

    ys = lax.map(expert_block, (xs, block_expert)).reshape(n_rows, d)
    slot_row = jnp.zeros((n_slots,), jnp.int32).at[order].set(dest.astype(jnp.int32))
    y_slots = ys[slot_row].reshape(n_tok, TOP_K, d)
    out = jnp.einsum('tk,tkd->td', gate_w, y_slots.astype(jnp.float32))
    return out.astype(h.dtype).reshape(b, s, d)


def _normal(key, shape, scale):
    return jax.random.normal(key, shape, jnp.float32) * scale


def setup_inputs(seed: int = 0) -> dict:
    key = jax.random.key(seed)
    ks = jax.random.split(key, 24)
    L = DEPTH
    beta = DEEPNORM_BETA
    x = jax.random.normal(ks[0], (BATCH, SEQ, D_MODEL), jnp.float32)
    offs = np.cumsum((0,) + IN_SPLITS)
    v0, v1 = int(offs[V_SPLIT_INDEX]), int(offs[V_SPLIT_INDEX + 1])
    w_in = _normal(ks[1], (L, D_MODEL, IN_DIM), D_MODEL ** -0.5)
    w_in = w_in.at[:, :, v0:v1].multiply(beta)
    conv_w = _normal(ks[2], (L, CONV_WIDTH, CONV_DIM), CONV_WIDTH ** -0.5)
    conv_b = _normal(ks[3], (L, CONV_DIM), 0.02)
    u = jax.random.uniform(ks[4], (L, SSM_HEADS), jnp.float32)
    dt0 = jnp.exp(u * (math.log(0.1) - math.log(0.001)) + math.log(0.001))
    dt_bias = dt0 + jnp.log(-jnp.expm1(-dt0))
    a_log = jnp.log(jax.random.uniform(ks[5], (L, SSM_HEADS), jnp.float32, minval=1.0, maxval=16.0))
    d_skip = 1.0 + _normal(ks[6], (L, SSM_HEADS), 0.1)
    ssm_norm_w = 1.0 + _normal(ks[7], (L, D_INNER), 0.02)
    w_ssm_out = _normal(ks[8], (L, D_INNER, D_MODEL), beta * D_INNER ** -0.5)
    attn_sinks = _normal(ks[9], (L, ATTN_HEADS), 0.5)
    w_attn_out = _normal(ks[10], (L, ATTN_HEADS * HEAD_DIM, D_MODEL), beta * (ATTN_HEADS * HEAD_DIM) ** -0.5)
    b_gates = _normal(ks[11], (L, N_BRANCHES * D_MODEL), 0.1)
    w_mix_out = _normal(ks[12], (L, D_MODEL, D_MODEL), beta * D_MODEL ** -0.5)
    ln1_g = 1.0 + _normal(ks[13], (L, D_MODEL), 0.02)
    ln1_b = _normal(ks[14], (L, D_MODEL), 0.02)
    w_router = _normal(ks[15], (L, D_MODEL, N_EXPERTS), D_MODEL ** -0.5)
    b_router = _normal(ks[16], (L, N_EXPERTS), 0.01)
    w_gate_up = _normal(ks[17], (L, N_EXPERTS, D_MODEL, 2 * D_FF), beta * D_MODEL ** -0.5)
    b_gate_up = _normal(ks[18], (L, N_EXPERTS, 2 * D_FF), 0.02)
    w_down = _normal(ks[19], (L, N_EXPERTS, D_FF, D_MODEL), beta * D_FF ** -0.5)
    b_down = _normal(ks[20], (L, N_EXPERTS, D_MODEL), 0.02)
    ln2_g = 1.0 + _normal(ks[21], (L, D_MODEL), 0.02)
    ln2_b = _normal(ks[22], (L, D_MODEL), 0.02)
    return {'x': x, 'w_in': w_in, 'conv_w': conv_w, 'conv_b': conv_b, 'dt_bias': dt_bias,
            'a_log': a_log, 'd_skip': d_skip, 'ssm_norm_w': ssm_norm_w, 'w_ssm_out': w_ssm_out,
            'attn_sinks': attn_sinks, 'w_attn_out': w_attn_out, 'b_gates': b_gates,
            'w_mix_out': w_mix_out, 'ln1_g': ln1_g, 'ln1_b': ln1_b, 'w_router': w_router,
            'b_router': b_router, 'w_gate_up': w_gate_up, 'b_gate_up': b_gate_up,
            'w_down': w_down, 'b_down': b_down, 'ln2_g': ln2_g, 'ln2_b': ln2_b}


def reference(x, w_in, conv_w, conv_b, dt_bias, a_log, d_skip, ssm_norm_w, w_ssm_out,
              attn_sinks, w_attn_out, b_gates, w_mix_out, ln1_g, ln1_b, w_router, b_router,
              w_gate_up, b_gate_up, w_down, b_down, ln2_g, ln2_b):
    h = x
    for i in range(DEPTH):
        mix = token_mixer(h, w_in[i], conv_w[i], conv_b[i], dt_bias[i], a_log[i], d_skip[i],
                          ssm_norm_w[i], w_ssm_out[i], attn_sinks[i], w_attn_out[i], b_gates[i],
                          w_mix_out[i])
        h = layer_norm(DEEPNORM_ALPHA * h + mix, ln1_g[i], ln1_b[i])
        ffn = moe_ffn(h, w_router[i], b_router[i], w_gate_up[i], b_gate_up[i], w_down[i], b_down[i])
        h = layer_norm(DEEPNORM_ALPHA * h + ffn, ln2_g[i], ln2_b[i])
    return h
```

```python
import contextlib
import numpy as np
import concourse.bass as bass
import concourse.mybir as mybir
from concourse.bass_utils import run_bass_kernel_spmd

F32 = mybir.dt.float32
BF16 = mybir.dt.bfloat16
I32 = mybir.dt.int32
AF = mybir.ActivationFunctionType
ALU = mybir.AluOpType
AX = mybir.AxisListType

NCORES = 8
NT = 4096
NCH = 32
NG = 8
CAP = 768
NEXP = 32
ALPHA = 2.0 ** 0.25
NEG = -30000.0
DEBUG_H = False
STOP = 0
NO_SCATTER = False
NGROUPS = NG
LIMIT = 0
DBGV = {}
MAXOPS = 0
NOBAR = False
LIMC = 4


class Buf:
    __slots__ = ("name", "w", "r", "excl")

    def __init__(self, name, excl=False):
        self.name = name
        self.w = None
        self.r = []
        self.excl = excl


class Prog:
    def __init__(self, nc, stack):
        self.nc = nc
        self.stack = stack
        self.ops = []
        self.sems = {}
        self.last = {}
        self.barbuf = {e: Buf("bar_" + e) for e in ("pe", "act", "dve", "pool")}
        self.barx = Buf("barx")
        self.pending = {}

    def op(self, eng, fn, reads=(), writes=(), dma=None, nowaw=False, force=False, final=False):
        if MAXOPS and len(self.ops) >= MAXOPS and not final:
            return len(self.ops) - 1
        i = len(self.ops)
        deps = set()
        for b in reads:
            if b.w is not None:
                deps.add(b.w)
            if b.excl:
                deps.update(r for r in b.r if self.ops[r]["eng"] != eng)
        for b in writes:
            if b.w is not None and not nowaw:
                deps.add(b.w)
            deps.update(b.r)
        for b in reads:
            b.r.append(i)
        for b in writes:
            b.w = i
            if not nowaw:
                b.r = []
        if self.pending.get(eng):
            deps.update(self.pending.pop(eng))
            force = True
        deps.discard(i)
        self.ops.append(dict(eng=eng, fn=fn, deps=deps, dma=dma, needed=False, token=None, waits=[], force=force))
        if dma is None:
            self.last[eng] = i
        return i

    def barrier(self, extra=()):
        if NOBAR:
            return
        ids = set(self.last.values())
        for b in extra:
            if b.w is not None:
                ids.add(b.w)
            ids.update(b.r)
        for e in ("pe", "act", "dve", "pool", "sp"):
            self.pending.setdefault(e, set()).update(ids)

    def _skip(self, op, D):
        return (D["dma"] is None and op["dma"] is None and D["eng"] == "pe" and op["eng"] == "pe"
                and not op["force"] and not D["force"])

    def finish(self):
        lastd = {}
        for i, op in enumerate(self.ops):
            if op["dma"] is not None:
                lastd[op["dma"]] = i
        lastv = list(self.last.values())
        i = self.op("sp", lambda en: en.nop(), force=True, final=True)
        self.ops[i]["deps"].update(lastd.values())
        self.ops[i]["deps"].update(lastv)
        self.ops[i]["deps"].discard(i)

    def run(self):
        self.finish()
        ops = self.ops
        for op in ops:
            for d in op["deps"]:
                if not self._skip(op, ops[d]):
                    ops[d]["needed"] = True
        cnt = {}
        for op in ops:
            if op["dma"] is not None:
                k = ("d", op["dma"])
                cnt[k] = cnt.get(k, 0) + 16
                op["token"] = (k, cnt[k])
            elif op["needed"]:
                k = ("e", op["eng"])
                cnt[k] = cnt.get(k, 0) + 1
                op["token"] = (k, cnt[k])
        waited = {}
        for op in ops:
            ws = {}
            for d in op["deps"]:
                D = ops[d]
                if self._skip(op, D):
                    continue
                k, v = D["token"]
                ws[k] = max(ws.get(k, 0), v)
            wd = waited.setdefault(op["eng"], {})
            op["waits"] = [(k, v) for k, v in ws.items() if wd.get(k, 0) < v]
            for k, v in op["waits"]:
                wd[k] = v
        for op in ops:
            if op["token"] and op["token"][0] not in self.sems:
                k = op["token"][0]
                self.sems[k] = self.stack.enter_context(self.nc.semaphore("s_%s_%s" % k))
        engs = {"pe": "tensor", "act": "scalar", "dve": "vector", "pool": "gpsimd", "sp": "sync"}
        with self.nc.Block() as block:
            for en, attr in engs.items():
                mine = [op for op in ops if op["eng"] == en]
                if not mine:
                    continue

                def body(e, mine=mine):
                    for op in mine:
                        for k, v in op["waits"]:
                            e.wait_ge(self.sems[k], v)
                        try:
                            ins = op["fn"](e)
                        except Exception:
                            print("FAILED OP eng", op["eng"], "dma", op["dma"], "nsems", len(self.sems), "free", self.nc.free_len())
                            raise
                        if op["token"]:
                            ins.then_inc(self.sems[op["token"][0]], 16 if op["dma"] is not None else 1)

                getattr(block, attr)(body)


def _mk(meth, *a, **k):
    return lambda e: getattr(e, meth)(*a, **k)


def v3(ap, b):
    return ap.rearrange("p (a b) -> p a b", b=b)


def bc(ap, m):
    p, n = ap.shape
    return ap.unsqueeze(2).to_broadcast([p, n, m])


def build_nc():
    nc = bass.Bass("TRN2", target_bir_lowering=False)
    din = lambda name, shape, dt=F32: nc.dram_tensor(name, list(shape), dt, kind="ExternalInput").ap()
    x_tok = din("x_tok", [NT, 1024])
    xT_d = din("xT", [1024, NT])
    w_fm = din("w_fm", [1024, 4608])
    w_z = din("w_z", [1024, 2048])
    w_vdt = din("w_vdt", [1024, 288])
    w_gp = din("w_gp", [1024, 2048])
    w_so = din("w_so", [2048, 1024])
    w_ao = din("w_ao", [1024, 1024])
    w_mix = din("w_mix", [1024, 1024])
    w_rt = din("w_rt", [1024, 32])
    NXD = 1 if STOP == 1 else NEXP
    w_gu = din("w_gu", [NXD, 1024, 2048])
    w_dn = din("w_dn", [NXD, 1024, 1024])
    bd_rep = din("bd_rep", [NXD, 128, 1024])
    convw_d = din("convw_l", [128, 24 * 4])
    convb_d = din("convb_l", [128, 24])
    bgate_d = din("bgate_l", [128, 16])
    normw_d = din("normw_l", [128, 16])
    bgu_d = din("bgu_l", [128, NEXP * 16])
    rep32_d = din("rep32", [128, 7 * 32])
    lnrep_d = din("ln_rep", [128, 4 * 1024])
    cst_d = din("cst", [128, 5 * 128])
    out_d = nc.dram_tensor("out", [NT, 1024], F32, kind="ExternalOutput").ap()
    hkind = "ExternalOutput" if DEBUG_H else "Internal"
    hbuf = nc.dram_tensor("hbuf", [NT, 1024], F32, kind=hkind).ap()
    Xg = nc.dram_tensor("Xg", [NEXP * CAP + 128, 1024], BF16, kind="Internal").ap()
    Yg = nc.dram_tensor("Yg", [NEXP * CAP + 128, 1024], F32, kind="Internal").ap()

    with contextlib.ExitStack() as st:
        P = Prog(nc, st)
        sbt = lambda name, shape, dt: st.enter_context(nc.sbuf_tensor("sb_" + name, list(shape), dt))
        banks = [st.enter_context(nc.psum_tensor("ps%d" % i, [128, 512], F32)) for i in range(8)]
        bbufs = [Buf("ps%d" % i, excl=True) for i in range(8)]
        bstate = [0]

        def pb():
            i = bstate[0] % 8
            bstate[0] += 1
            return banks[i], bbufs[i]

        cst = sbt("cst", [128, 640], F32)
        ident_f = cst[:, 0:128]
        tri = cst[:, 128:256]
        cstb = sbt("cstb", [128, 640], BF16)
        ident_b = cstb[:, 0:128]
        stri_b = cstb[:, 256:384]
        ones_f = sbt("ones_f", [128, 128], F32)
        ones_b = sbt("ones_b", [128, 128], BF16)
        negm = sbt("negm", [128, 2, 512], BF16)
        convw = sbt("convw", [128, 24, 4], F32)
        convb = sbt("convb", [128, 24], F32)
        bgate = sbt("bgate", [128, 16], F32)
        normw = sbt("normw", [128, 16], F32)
        bgu = sbt("bgu", [128, NEXP, 16], F32)
        rep32 = sbt("rep32", [128, 7, 32], F32)
        lnrep = sbt("lnrep", [128, 4, 1024], F32)
        A_rep = sbt("A_rep", [128, 32], F32)
        esink = sbt("esink", [128, 16], F32)
        cnt = sbt("cnt", [128, 32], F32)
        dest2 = sbt("dest", [128, NCH * 4], I32)
        dest = v3(dest2[:, :], 4)
        gatew = sbt("gatew", [128, NCH, 4], F32)
        wrt = sbt("wrt", [128, 8, 32], F32)
        wvdt = sbt("wvdt", [128, 8, 288], BF16)
        B = {}

        def bf(n):
            if n not in B:
                B[n] = Buf(n)
            return B[n]

        ld = lambda dst, src, name: P.op("sp", _mk("dma_start", out=dst, in_=src), writes=[bf(name)], dma="c_" + name)
        ld(cst[:, :], cst_d, "cst")
        ld(convw[:, :, :], v3(convw_d, 4), "convw")
        ld(convb[:, :], convb_d, "convb")
        ld(bgate[:, :], bgate_d, "bgate")
        ld(normw[:, :], normw_d, "normw")
        ld(bgu[:, :, :], v3(bgu_d, 16), "bgu")
        ld(rep32[:, :, :], v3(rep32_d, 32), "rep32")
        ld(lnrep[:, :, :], v3(lnrep_d, 1024), "lnrep")
        ld(wrt[:, :, :], w_rt.rearrange("(kt p) c -> p kt c", p=128), "wrt")
        P.op("pool", _mk("dma_start", out=wvdt[:, :, :], in_=w_vdt.rearrange("(kt p) c -> p kt c", p=128)),
             writes=[bf("wvdt")], dma="c_wvdt")
        P.op("dve", _mk("tensor_copy", out=cstb[:, :], in_=cst[:, :]), reads=[bf("cst")], writes=[bf("cstb")])
        P.op("dve", _mk("memset", ones_f[:, :], 1.0), writes=[bf("ones_f")])
        P.op("dve", _mk("memset", ones_b[:, :], 1.0), writes=[bf("ones_b")])
        P.op("dve", _mk("memset", cnt[:, :], 0.0), writes=[bf("cnt")])
        for kt_ in range(2):
            for r_ in range(4):
                c0_ = 512 if kt_ == 0 else 384
                P.op("dve", _mk("tensor_copy",
                    out=negm[:, kt_, r_ * 128:(r_ + 1) * 128], in_=cst[:, c0_:c0_ + 128]),
                    reads=[bf("cst")], writes=[bf("negm")])
        P.op("act", _mk("activation", out=A_rep[:, :], in_=rep32[:, 1, :], func=AF.Exp), reads=[bf("rep32")], writes=[bf("A_rep")])
        P.op("dve", _mk("tensor_scalar", out=A_rep[:, :], in0=A_rep[:, :], scalar1=-1.0, scalar2=0.0, op0=ALU.mult, op1=ALU.add),
             reads=[bf("A_rep")], writes=[bf("A_rep")])
        P.op("act", _mk("activation", out=esink[:, :], in_=rep32[:, 4, 0:16], func=AF.Exp), reads=[bf("rep32")], writes=[bf("esink")])
        dtb_rep = rep32[:, 0, :]
        D_rep = rep32[:, 2, :]
        brt_rep = rep32[:, 3, :]
        ebase = rep32[:, 5, :]
        dumpT = rep32[:, 6, :]
        CB = [bf("cst"), bf("cstb"), bf("negm"), bf("ones_f"), bf("ones_b")]

        ARW = 43200
        arena = sbt("arena", [128, ARW], F32)
        aoff = [0]

        def alloc(shape, dt, name=None):
            if name:
                DBGV[name] = (aoff[0], tuple(shape), dt)
            n = int(np.prod(shape[1:]))
            words = n if dt in (F32, I32) else (n + 1) // 2
            words = (words + 7) // 8 * 8
            v = arena[0:shape[0], aoff[0]:aoff[0] + words]
            aoff[0] += words
            assert aoff[0] <= ARW, aoff[0]
            if dt != F32:
                v = v.bitcast(dt)
            v = v[:, 0:n]
            if len(shape) == 3:
                v = v3(v, shape[2])
            elif len(shape) == 4:
                v = v.rearrange("p (a b c) -> p a b c", b=shape[2], c=shape[3])
            return v

        xTb = alloc([128, 8, 512], BF16, "xTb")
        slabs = [alloc([128, 4096], BF16) for _ in range(3)]
        sl_b = [Buf("slab%d" % i) for i in range(3)]
        slst = [0]
        xbc = alloc([128, 24, 512], BF16, "xbc")
        QT = alloc([128, 2, 8, 512], BF16, "QT")
        KT = alloc([128, 4, 640], BF16, "KT")
        Vaug = alloc([128, 2, 4, 68], BF16, "Vaug")
        sz = alloc([128, 4, 2048], BF16, "sz")
        hist = alloc([128, 24, 3], F32)
        S = alloc([128, 2048], F32, "S")
        Sbf = alloc([128, 2048], BF16)
        region0 = aoff[0]
        U = [alloc([128, 515], F32) for _ in range(2)]
        acc = [alloc([128, 512], F32) for _ in range(2)]
        aoff[0] = region0
        sm = alloc([128, 12, 32], F32, "sm")
        xtok = alloc([128, 2048], BF16, "xtok")
        xs = alloc([128, 2048], BF16, "xs")
        xsw = alloc([128, 2048], BF16)
        Btok = alloc([128, 4, 128], BF16, "Btok")
        cbT = alloc([128, 4, 128], F32, "cbT")
        LT = [alloc([128, 512], F32) for _ in range(2)]
        MT = [alloc([128, 512], BF16) for _ in range(4)]
        t1 = [alloc([128, 512], F32) for _ in range(2)]
        t2 = [alloc([128, 512], F32) for _ in range(2)]
        yz = alloc([128, 2048], F32, "yz")
        junk = alloc([128, 1024], F32)
        yn = alloc([128, 2048], BF16, "yn")
        ss = alloc([128, 8], F32)
        PT = [alloc([128, 2, 512], BF16) for _ in range(2)]
        atok = alloc([128, 1024], BF16, "atok")
        rec = alloc([128, 8], F32)
        regionC_end = aoff[0]
        aoff[0] = region0
        mergedT = alloc([128, 8, 512], BF16, "mergedT")
        g1m = [alloc([128, 2, 512], F32) for _ in range(2)]
        g0t = [alloc([128, 2, 512], F32) for _ in range(2)]
        xt32 = alloc([128, 1024], F32)
        hh = alloc([128, 1024], F32, "hh")
        hbf = alloc([128, 1024], BF16)
        hT = alloc([128, 8, 128], F32)
        st4 = alloc([128, 16], F32)
        lg = alloc([128, 32], F32)
        oh = alloc([128, 4, 32], F32)
        Mm = alloc([128, 32], F32)
        Mb = alloc([128, 32], BF16)
        Tt = alloc([128, 3, 32], F32)
        destf = alloc([128, 4], F32)
        exv = alloc([128, 4], F32)
        regionDE_end = aoff[0]
        p1_end = max(regionC_end, regionDE_end)
        ynT = xbc[:, 0:16, :]
        aT = xbc[:, 16:24, :]

        plan = []
        issued = [0]

        def slab_issue_upto(n):
            while issued[0] < min(n, len(plan)):
                i = issued[0]
                src_ap, (a, b) = plan[i]
                view = v3(slabs[i % 3][:, 0:a * b], b)
                P.op("pool", _mk("dma_start", out=view, in_=src_ap),
                     writes=[sl_b[i % 3]], dma="slab%d" % (i % 3))
                issued[0] += 1

        def slab_load(src_ap, shape3):
            i = slst[0]
            slst[0] += 1
            assert plan[i][1] == shape3, (i, plan[i][1], shape3)
            slab_issue_upto(i + 2)
            a, b = shape3
            return v3(slabs[i % 3][:, 0:a * b], b), sl_b[i % 3]

        def mm(out, lhsT, rhs, start, stop, reads, wbuf):
            P.op("pe", _mk("matmul", out, lhsT=lhsT, rhs=rhs, start=start, stop=stop), reads=reads, writes=[wbuf])

        def tr(out, in_, ident, reads, wbuf):
            P.op("pe", _mk("transpose", out=out, in_=in_, identity=ident), reads=reads, writes=[wbuf])

        wfm_v = w_fm.rearrange("(kt p) c -> p kt c", p=128)
        wz_v = w_z.rearrange("(kt p) c -> p kt c", p=128)
        wgp_v = w_gp.rearrange("(kt p) c -> p kt c", p=128)
        wso_v = w_so.rearrange("(kt p) c -> p kt c", p=128)
        wao_v = w_ao.rearrange("(kt p) c -> p kt c", p=128)
        wmix_v = w_mix.rearrange("(kt p) c -> p kt c", p=128)
        xT_v = xT_d.rearrange("(kt p) t -> p kt t", p=128)
        for g_ in range(NGROUPS):
            for sl in range(9):
                plan.append((wfm_v[:, :, sl * 512:(sl + 1) * 512], (8, 512)))
            for zs in range(4):
                plan.append((wz_v[:, :, zs * 512:(zs + 1) * 512], (8, 512)))
            for pi in range(4):
                plan.append((wgp_v[:, :, pi * 512:(pi + 1) * 512], (8, 512)))
                plan.append((wao_v[:, :, pi * 256:(pi + 1) * 256], (8, 256)))
                plan.append((wso_v[:, :, pi * 256:(pi + 1) * 256], (16, 256)))
            for hf in range(2):
                plan.append((wmix_v[:, :, hf * 512:(hf + 1) * 512], (8, 512)))

        bXT = bf("xT")
        bXx = [bf("xbc_x%d" % i) for i in range(4)]
        bXbc = [bf("xbc_bc%d" % i) for i in range(4)]
        bQT, bKT, bV = bf("QT"), bf("KT"), [bf("V0"), bf("V1")]
        bSZ = [bf("sz%d" % i) for i in range(4)]
        bHist = [bf("hist%d" % i) for i in range(24)]
        bS = [bf("S%d" % i) for i in range(4)]
        bSb = [bf("Sb%d" % i) for i in range(4)]
        bU = [bf("U0"), bf("U1")]
        bAcc = [bf("acc0"), bf("acc1")]
        PRM = [bf("convw"), bf("convb")]
        bOUT = bf("OUT")
        bHB = bf("hbufd")
        bXG = bf("Xgd")
        bYG = bf("Ygd")
        bDest = bf("dest")
        bGw = bf("gatew")
        P.op("dve", _mk("memset", Vaug[:, :, :, :], 1.0), writes=bV)
        P.op("dve", _mk("memset", QT[:, :, :, :], 0.0), writes=[bQT])
        ztile = sbt("ztile", [128, 1024], BF16)
        P.op("dve", _mk("memset", ztile[:, :], 0.0), writes=[bf("ztile")])
        for r0_ in range(0, NEXP * CAP + 128, 128):
            P.op("sp", _mk("dma_start", out=Xg[r0_:r0_ + 128, :], in_=ztile[:, :]), reads=[bf("ztile")], writes=[bXG], dma="xgz", nowaw=True)
        first_sc = [True]

        for g in range(NGROUPS):
            gi = g % 4
            first_grp = gi == 0
            tok0 = g * 512
            P.op("pool", _mk("dma_start", out=xTb[:, :, :], in_=xT_v[:, :, tok0:tok0 + 512]),
                 writes=[bXT], dma="xT")
            if first_grp:
                P.op("dve", _mk("memset", S[:, :], 0.0), writes=bS)
                P.op("dve", _mk("memset", Sbf[:, :], 0.0), writes=bSb)
            else:
                P.op("pool", _mk("tensor_copy", out=KT[:, :, 0:128], in_=KT[:, :, 512:640]), reads=[bKT], writes=[bKT])
            for sl in range(9):
                wv, wb_ = slab_load(wfm_v[:, :, sl * 512:(sl + 1) * 512], (8, 512))
                for t4 in range(4):
                    ti = sl * 4 + t4
                    bk, bb = pb()
                    for kt in range(8):
                        mm(bk[:, :], wv[:, kt, t4 * 128:(t4 + 1) * 128], xTb[:, kt, :], kt == 0, kt == 7, [wb_, bXT], bb)
                    if ti < 24:
                        u = ti % 2
                        Uu, au = U[u], acc[u]
                        P.op("dve", _mk("tensor_copy", out=Uu[:, 3:515], in_=bk[:, :]), reads=[bb], writes=[bU[u]])
                        if first_grp:
                            P.op("pool", _mk("memset", Uu[:, 0:3], 0.0), writes=[bU[u]])
                        else:
                            P.op("pool", _mk("tensor_copy", out=Uu[:, 0:3], in_=hist[:, ti, :]),
                                 reads=[bHist[ti]], writes=[bU[u]])
                        P.op("dve", _mk("tensor_scalar",
                            out=au[:, :], in0=bk[:, :], scalar1=convw[:, ti, 3:4], scalar2=convb[:, ti:ti + 1], op0=ALU.mult, op1=ALU.add),
                            reads=[bb] + PRM, writes=[bAcc[u]])
                        for j in range(3):
                            P.op("dve", _mk("scalar_tensor_tensor",
                                out=au[:, :], in0=Uu[:, j:j + 512], scalar=convw[:, ti, j:j + 1], in1=au[:, :],
                                op0=ALU.mult, op1=ALU.add), reads=[bU[u], bAcc[u]] + PRM, writes=[bAcc[u]])
                        wl = bXx if ti < 16 else bXbc
                        P.op("act", _mk("activation", out=xbc[:, ti, :], in_=au[:, :], func=AF.Silu),
                             reads=[bAcc[u]], writes=wl)
                        P.op("pool", _mk("tensor_copy", out=hist[:, ti, :], in_=Uu[:, 512:515]),
                             reads=[bU[u]], writes=[bHist[ti]])
                    elif ti < 32:
                        P.op("act", _mk("activation", func=AF.Copy, out=QT[0:64, 0, ti - 24, :], in_=bk[0:64, :]), reads=[bb], writes=[bQT])
                        P.op("act", _mk("activation", func=AF.Copy, out=QT[64:128, 1, ti - 24, :], in_=bk[64:128, :]), reads=[bb], writes=[bQT])
                    else:
                        P.op("dve", _mk("tensor_copy", out=KT[:, ti - 32, 128:640], in_=bk[:, :]), reads=[bb], writes=[bKT])
            for zs in range(4):
                wv, wb_ = slab_load(wz_v[:, :, zs * 512:(zs + 1) * 512], (8, 512))
                for ch in range(4):
                    bk, bb = pb()
                    for kt in range(8):
                        mm(bk[:, :], xTb[:, kt, ch * 128:(ch + 1) * 128], wv[:, kt, :], kt == 0, kt == 7, [wb_, bXT], bb)
                    P.op("act", _mk("activation",
                        out=sz[:, ch, zs * 512:(zs + 1) * 512], in_=bk[:, :], func=AF.Silu), reads=[bb], writes=[bSZ[ch]])
            P.barrier()
            if LIMIT == 1:
                break
            for ch in range(LIMC if LIMIT == 2 else 4):
                c = g * 4 + ch
                ci = c % 16
                cols = slice(ch * 128, (ch + 1) * 128)
                cur, prv = c % 2, (c + 1) % 2
                tS, tm_, el, dtv, av, nac, ea, dd, dte, cd = [sm[:, i, :] for i in range(10)]
                bsm = bf("sm")
                bk, bb = pb()
                for kt in range(8):
                    mm(bk[:, 0:288], xTb[:, kt, cols], wvdt[:, kt, :], kt == 0, kt == 7, [bXT, bf("wvdt")], bb)
                P.op("dve", _mk("tensor_copy", out=Vaug[:, cur, :, 0:64], in_=v3(bk[:, 0:256], 64)), reads=[bb], writes=[bV[cur]])
                P.op("dve", _mk("tensor_tensor", out=tS, in0=bk[:, 256:288], in1=dtb_rep, op=ALU.add), reads=[bb, bf("rep32")], writes=[bsm])
                P.op("dve", _mk("tensor_scalar_min", out=tm_, in0=tS, scalar1=30.0), reads=[bsm], writes=[bsm])
                P.op("act", _mk("activation", out=el, in_=tm_, func=AF.Exp), reads=[bsm], writes=[bsm])
                P.op("act", _mk("activation", out=el, in_=el, func=AF.Ln, bias=1.0), reads=[bsm], writes=[bsm])
                P.op("dve", _mk("tensor_max", out=dtv, in0=tS, in1=el), reads=[bsm], writes=[bsm])
                P.op("dve", _mk("tensor_tensor", out=av, in0=dtv, in1=A_rep[:, :], op=ALU.mult), reads=[bsm, bf("A_rep")], writes=[bsm])
                bk2, bb2 = pb()
                mm(bk2[:, 0:32], tri, av, True, True, [bsm] + CB, bb2)
                mm(bk2[:, 32:64], ones_f[:, :], av, True, True, [bsm] + CB, bb2)
                P.op("dve", _mk("tensor_scalar", out=nac, in0=bk2[:, 0:32], scalar1=-1.0, scalar2=0.0, op0=ALU.mult, op1=ALU.add),
                     reads=[bb2], writes=[bsm])
                P.op("act", _mk("activation", out=ea, in_=bk2[:, 0:32], func=AF.Exp), reads=[bb2], writes=[bsm])
                P.op("dve", _mk("tensor_tensor", out=dd, in0=bk2[:, 32:64], in1=nac, op=ALU.add), reads=[bb2, bsm], writes=[bsm])
                P.op("act", _mk("activation", out=dte, in_=dd, func=AF.Exp), reads=[bsm], writes=[bsm])
                P.op("act", _mk("activation", out=cd, in_=bk2[:, 32:64], func=AF.Exp), reads=[bb2], writes=[bsm])
                bXs, bXtok, bXsw, bBtok = bf("xs"), bf("xtok"), bf("xsw"), bf("Btok")
                for bi in range(2):
                    bk, bb = pb()
                    pbf = bk[:, :].bitcast(BF16)
                    for j in range(8):
                        tr(pbf[:, j * 128:(j + 1) * 128], xbc[:, bi * 8 + j, cols], ident_b, [bXx[ch]] + CB, bb)
                    P.op("act", _mk("activation", func=AF.Copy, out=xtok[:, bi * 1024:(bi + 1) * 1024], in_=pbf), reads=[bb], writes=[bXtok])
                    P.op("dve", _mk("tensor_tensor",
                        out=v3(xs[:, bi * 1024:(bi + 1) * 1024], 64), in0=v3(pbf, 64), in1=bc(dtv[:, bi * 16:(bi + 1) * 16], 64), op=ALU.mult),
                        reads=[bb, bsm, bXtok], writes=[bXs])
                P.op("dve", _mk("tensor_tensor", out=v3(xsw[:, :], 64), in0=v3(xs[:, :], 64), in1=bc(dte, 64), op=ALU.mult),
                     reads=[bXs, bsm], writes=[bXsw])
                bk, bb = pb()
                pbf = bk[:, :].bitcast(BF16)
                for gg in range(4):
                    tr(pbf[:, gg * 128:(gg + 1) * 128], xbc[:, 16 + gg, cols], ident_b, [bXbc[ch]] + CB, bb)
                P.op("act", _mk("activation", func=AF.Copy, out=Btok[:, :, :], in_=v3(pbf[:, 0:512], 128)), reads=[bb], writes=[bBtok])
                bk, bb = pb()
                for gg in range(4):
                    mm(bk[:, gg * 128:(gg + 1) * 128], xbc[:, 16 + gg, cols], xbc[:, 20 + gg, cols], True, True, [bXbc[ch]], bb)
                bcb = bf("cbT")
                P.op("act", _mk("activation", func=AF.Copy, out=cbT[:, :, :], in_=v3(bk[:, :], 128)), reads=[bb], writes=[bcb])
                byz = bf("yz")
                bss = bf("ss")
                P.op("dve", _mk("memset", ss[:, :], 0.0), writes=[bss])
                for gg in range(4):
                    bkA, bbA = pb()
                    for hf in range(2):
                        bkR, bbR = pb()
                        li = (gg * 2 + hf) % 2
                        mi = (gg * 2 + hf) % 4
                        bLT, bMT = bf("LT%d" % li), bf("MT%d" % mi)
                        for j in range(4):
                            h = 8 * gg + 4 * hf + j
                            mm(bkR[:, j * 128:(j + 1) * 128], ident_b, negm[:, 1, 0:128], True, False, CB, bbR)
                            mm(bkR[:, j * 128:(j + 1) * 128], av[:, h:h + 1].to_broadcast([128, 128]), tri, False, True, [bsm] + CB, bbR)
                        for j in range(4):
                            h = 8 * gg + 4 * hf + j
                            P.op("act", _mk("activation",
                                out=LT[li][:, j * 128:(j + 1) * 128], in_=bkR[:, j * 128:(j + 1) * 128], func=AF.Exp, bias=nac[:, h:h + 1]),
                                reads=[bbR, bsm], writes=[bLT])
                        P.op("dve", _mk("tensor_tensor",
                            out=v3(MT[mi][:, :], 128), in0=v3(LT[li][:, :], 128), in1=cbT[:, gg:gg + 1, :].to_broadcast([128, 4, 128]), op=ALU.mult),
                            reads=[bLT, bcb], writes=[bMT])
                        for j in range(4):
                            h = 8 * gg + 4 * hf + j
                            mm(bkA[:, (4 * hf + j) * 64:(4 * hf + j + 1) * 64], MT[mi][:, j * 128:(j + 1) * 128], xs[:, h * 64:(h + 1) * 64],
                               True, True, [bMT, bXs], bbA)
                    gs = slice(gg * 512, (gg + 1) * 512)
                    bkB, bbB = pb()
                    mm(bkB[:, :], xbc[:, 20 + gg, cols], Sbf[:, gs], True, True, [bXbc[ch], bSb[gg]], bbB)
                    bkC, bbC = pb()
                    mm(bkC[:, :], Btok[:, gg, :], xsw[:, gs], True, True, [bBtok, bXsw], bbC)
                    ti_ = gg % 2
                    bt1, bt2 = bf("t1_%d" % ti_), bf("t2_%d" % ti_)
                    P.op("dve", _mk("tensor_tensor",
                        out=v3(t1[ti_][:, :], 64), in0=v3(bkB[:, :], 64), in1=bc(ea[:, 8 * gg:8 * gg + 8], 64), op=ALU.mult),
                        reads=[bbB, bsm], writes=[bt1])
                    P.op("pool", _mk("tensor_tensor",
                        out=v3(t2[ti_][:, :], 64), in0=v3(xtok[:, gs], 64), in1=bc(D_rep[:, 8 * gg:8 * gg + 8], 64), op=ALU.mult),
                        reads=[bXtok, bf("rep32")], writes=[bt2])
                    P.op("pool", _mk("tensor_tensor", out=t1[ti_][:, :], in0=t1[ti_][:, :], in1=t2[ti_][:, :], op=ALU.add),
                         reads=[bt1, bt2], writes=[bt1])
                    P.op("dve", _mk("tensor_tensor", out=yz[:, gs], in0=bkA[:, :], in1=t1[ti_][:, :], op=ALU.add),
                         reads=[bbA, bt1], writes=[byz])
                    P.op("dve", _mk("tensor_tensor", out=yz[:, gs], in0=yz[:, gs], in1=sz[:, ch, gs], op=ALU.mult),
                         reads=[byz, bSZ[ch]], writes=[byz])
                    P.op("act", _mk("activation", out=junk[:, 0:512], in_=yz[:, gs], func=AF.Square, accum_out=ss[:, gg:gg + 1]),
                         reads=[byz, bss], writes=[bss, bf("junk")])
                    P.op("pool", _mk("tensor_tensor",
                        out=v3(S[:, gs], 64), in0=v3(S[:, gs], 64), in1=bc(cd[:, 8 * gg:8 * gg + 8], 64), op=ALU.mult),
                        reads=[bS[gg], bsm], writes=[bS[gg]])
                    P.op("dve", _mk("tensor_tensor", out=S[:, gs], in0=S[:, gs], in1=bkC[:, :], op=ALU.add),
                         reads=[bS[gg], bbC], writes=[bS[gg]])
                    P.op("act", _mk("activation", func=AF.Copy, out=Sbf[:, gs], in_=S[:, gs]), reads=[bS[gg]], writes=[bSb[gg]])
                P.op("dve", _mk("tensor_scalar", out=ss[:, 4:8], in0=ss[:, 0:4], scalar1=1.0 / 512.0, scalar2=1e-5, op0=ALU.mult, op1=ALU.add),
                     reads=[bss], writes=[bss])
                P.op("act", _mk("activation", out=ss[:, 4:8], in_=ss[:, 4:8], func=AF.Ln), reads=[bss], writes=[bss])
                P.op("act", _mk("activation", out=ss[:, 4:8], in_=ss[:, 4:8], func=AF.Exp, scale=-0.5), reads=[bss], writes=[bss])
                byn = bf("yn")
                for gg in range(4):
                    gs = slice(gg * 512, (gg + 1) * 512)
                    P.op("dve", _mk("tensor_scalar", out=yn[:, gs], in0=yz[:, gs], scalar1=ss[:, 4 + gg:5 + gg], scalar2=0.0, op0=ALU.mult, op1=ALU.add),
                         reads=[byz, bss], writes=[byn])
                for bi in range(2):
                    bk, bb = pb()
                    pbf = bk[:, :].bitcast(BF16)
                    for j in range(8):
                        tr(pbf[:, j * 128:(j + 1) * 128], yn[:, (bi * 8 + j) * 128:(bi * 8 + j + 1) * 128], ident_b, [byn] + CB, bb)
                    P.op("dve", _mk("tensor_tensor",
                        out=ynT[:, bi * 8:(bi + 1) * 8, cols], in0=v3(pbf, 128), in1=bc(normw[:, bi * 8:(bi + 1) * 8], 128), op=ALU.mult),
                        reads=[bb, bf("normw")], writes=[bXx[ch]])
                kts = [1] if ci == 0 else [0, 1]
                bat = bf("atok")
                brec = bf("rec")
                for gg in range(4):
                    pi_ = gg % 2
                    bPT = bf("PT%d" % pi_)
                    for kt in kts:
                        kcols = slice((ch + kt) * 128, (ch + kt + 1) * 128)
                        bk, bb = pb()
                        for j in range(4):
                            js = slice(j * 128, (j + 1) * 128)
                            mm(bk[:, js], ident_b, negm[:, kt, 0:128], True, False, CB, bb)
                            mm(bk[:, js], KT[:, gg, kcols], QT[:, j % 2, 2 * gg + j // 2, cols], False, True, [bKT, bQT], bb)
                        P.op("act", _mk("activation", out=PT[pi_][:, kt, :], in_=bk[:, :], func=AF.Exp, scale=0.125),
                             reads=[bb], writes=[bPT])
                    bkO, bbO = pb()
                    for jj in range(4):
                        j = jj
                        for kt in kts:
                            blk = cur if kt == 1 else prv
                            mm(bkO[:, j * 65:(j + 1) * 65], PT[pi_][:, kt, jj * 128:(jj + 1) * 128], Vaug[:, blk, gg, 0:65],
                               kt == kts[0], kt == kts[-1], [bPT, bV[blk]], bbO)
                    O3 = v3(bkO[:, 0:260], 65)
                    P.op("dve", _mk("tensor_tensor", out=rec[:, 0:4], in0=O3[:, :, 64], in1=esink[:, 4 * gg:4 * gg + 4], op=ALU.add),
                         reads=[bbO, bf("esink")], writes=[brec])
                    P.op("dve", _mk("reciprocal", out=rec[:, 4:8], in_=rec[:, 0:4]), reads=[brec], writes=[brec])
                    P.op("dve", _mk("tensor_tensor",
                        out=v3(atok[:, gg * 256:(gg + 1) * 256], 64), in0=O3[:, :, 0:64], in1=bc(rec[:, 4:8], 64), op=ALU.mult),
                        reads=[bbO, brec], writes=[bat])
                bk, bb = pb()
                pbf = bk[:, :].bitcast(BF16)
                for j in range(8):
                    tr(pbf[:, j * 128:(j + 1) * 128], atok[:, j * 128:(j + 1) * 128], ident_b, [bat] + CB, bb)
                P.op("act", _mk("activation", func=AF.Copy, out=aT[:, :, cols], in_=v3(pbf, 128)), reads=[bb], writes=[bXbc[ch]])
            P.barrier()
            if LIMIT == 2:
                break
            bMg = bf("mergedT")
            for pi in range(4):
                ri = pi % 2
                bG1, bG0 = bf("g1m%d" % ri), bf("g0t%d" % ri)
                wv, wb_ = slab_load(wgp_v[:, :, pi * 512:(pi + 1) * 512], (8, 512))
                gb = []
                for q4 in range(4):
                    bk, bb = pb()
                    for kt in range(8):
                        mm(bk[:, :], wv[:, kt, q4 * 128:(q4 + 1) * 128], xTb[:, kt, :], kt == 0, kt == 7, [wb_, bXT], bb)
                    i2 = q4 % 2
                    if q4 < 2:
                        P.op("act", _mk("activation",
                            out=g0t[ri][:, i2, :], in_=bk[:, :], func=AF.Sigmoid, bias=bgate[:, 2 * pi + i2:2 * pi + i2 + 1]),
                            reads=[bb, bf("bgate")], writes=[bG0])
                    else:
                        P.op("act", _mk("activation",
                            out=g1m[ri][:, i2, :], in_=bk[:, :], func=AF.Sigmoid, bias=bgate[:, 8 + 2 * pi + i2:8 + 2 * pi + i2 + 1]),
                            reads=[bb, bf("bgate")], writes=[bG1])
                wv, wb_ = slab_load(wao_v[:, :, pi * 256:(pi + 1) * 256], (8, 256))
                for i2 in range(2):
                    bk, bb = pb()
                    for kt in range(8):
                        mm(bk[:, :], wv[:, kt, i2 * 128:(i2 + 1) * 128], aT[:, kt, :], kt == 0, kt == 7, [wb_] + bXbc, bb)
                    P.op("dve", _mk("tensor_tensor", out=g1m[ri][:, i2, :], in0=bk[:, :], in1=g1m[ri][:, i2, :], op=ALU.mult),
                         reads=[bb, bG1], writes=[bG1])
                wv, wb_ = slab_load(wso_v[:, :, pi * 256:(pi + 1) * 256], (16, 256))
                for i2 in range(2):
                    bk, bb = pb()
                    for kt in range(16):
                        mm(bk[:, :], wv[:, kt, i2 * 128:(i2 + 1) * 128], ynT[:, kt, :], kt == 0, kt == 15, [wb_] + bXx, bb)
                    P.op("dve", _mk("tensor_tensor", out=g0t[ri][:, i2, :], in0=bk[:, :], in1=g0t[ri][:, i2, :], op=ALU.mult),
                         reads=[bb, bG0], writes=[bG0])
                    P.op("pool", _mk("tensor_tensor",
                        out=mergedT[:, 2 * pi + i2, :], in0=g0t[ri][:, i2, :], in1=g1m[ri][:, i2, :], op=ALU.add),
                        reads=[bG0, bG1], writes=[bMg])
            if LIMIT == 3:
                break
            wmx = []
            for hf in range(2):
                wmx.append(slab_load(wmix_v[:, :, hf * 512:(hf + 1) * 512], (8, 512)))
            bXt, bH, bHbf, bHT, bSt = bf("xt32"), bf("hh"), bf("hbf"), bf("hT"), bf("st4")
            bLg, bOh, bMm, bMb, bTt, bDf, bEx = bf("lg"), bf("oh"), bf("Mm"), bf("Mb"), bf("Tt"), bf("destf"), bf("exv")
            for ch in range(4):
                c = g * 4 + ch
                cols = slice(ch * 128, (ch + 1) * 128)
                rows = slice(c * 128, (c + 1) * 128)
                P.op("sp", _mk("dma_start", out=xt32[:, :], in_=x_tok[rows, :]), writes=[bXt], dma="xt32")
                for hf in range(2):
                    wv, wb_ = wmx[hf]
                    bk, bb = pb()
                    for kt in range(8):
                        mm(bk[:, :], mergedT[:, kt, cols], wv[:, kt, :], kt == 0, kt == 7, [bMg, wb_], bb)
                    hs = slice(hf * 512, (hf + 1) * 512)
                    P.op("dve", _mk("scalar_tensor_tensor",
                        out=hh[:, hs], in0=xt32[:, hs], scalar=ALPHA, in1=bk[:, :], op0=ALU.mult, op1=ALU.add),
                        reads=[bXt, bb], writes=[bH])
                emit_ln(P, hh, st4, hT, bH, bSt, bHT, lnrep[:, 0, :], lnrep[:, 1, :], bf("lnrep"))
                P.op("sp", _mk("dma_start", out=hbuf[rows, :], in_=hh[:, :]), reads=[bH], writes=[bHB], dma="hst", nowaw=True)
                P.op("act", _mk("activation", func=AF.Copy, out=hbf[:, :], in_=hh[:, :]), reads=[bH], writes=[bHbf])
                for b2 in range(2):
                    bk, bb = pb()
                    for j in range(4):
                        jj = b2 * 4 + j
                        tr(bk[:, j * 128:(j + 1) * 128], hh[:, jj * 128:(jj + 1) * 128], ident_f, [bH] + CB, bb)
                    P.op("act", _mk("activation", func=AF.Copy, out=hT[:, b2 * 4:(b2 + 1) * 4, :], in_=v3(bk[:, :], 128)), reads=[bb], writes=[bHT])
                bk, bb = pb()
                for kt in range(8):
                    mm(bk[:, 0:32], hT[:, kt, :], wrt[:, kt, :], kt == 0, kt == 7, [bHT, bf("wrt")], bb)
                P.op("dve", _mk("tensor_tensor", out=lg[:, :], in0=bk[:, 0:32], in1=brt_rep, op=ALU.add), reads=[bb, bf("rep32")], writes=[bLg])
                for k in range(4):
                    P.op("dve", _mk("reduce_max", out=st4[:, 8 + k:9 + k], in_=lg[:, :], axis=AX.X), reads=[bLg], writes=[bSt])
                    P.op("dve", _mk("tensor_scalar", out=oh[:, k, :], in0=lg[:, :], scalar1=st4[:, 8 + k:9 + k], scalar2=0.0,
                                                               op0=ALU.is_equal, op1=ALU.add), reads=[bLg, bSt], writes=[bOh])
                    P.op("dve", _mk("scalar_tensor_tensor", out=lg[:, :], in0=oh[:, k, :], scalar=-1e9, in1=lg[:, :], op0=ALU.mult, op1=ALU.add),
                         reads=[bOh, bLg], writes=[bLg])
                P.op("dve", _mk("tensor_tensor", out=Mm[:, :], in0=oh[:, 0, :], in1=oh[:, 1, :], op=ALU.add), reads=[bOh], writes=[bMm])
                P.op("dve", _mk("tensor_tensor", out=Mm[:, :], in0=Mm[:, :], in1=oh[:, 2, :], op=ALU.add), reads=[bOh, bMm], writes=[bMm])
                P.op("dve", _mk("tensor_tensor", out=Mb[:, :], in0=Mm[:, :], in1=oh[:, 3, :], op=ALU.add), reads=[bOh, bMm], writes=[bMb])
                P.op("dve", _mk("tensor_scalar", out=st4[:, 13:14], in0=st4[:, 8:9], scalar1=-1.0, scalar2=0.0, op0=ALU.mult, op1=ALU.add),
                     reads=[bSt], writes=[bSt])
                P.op("dve", _mk("memset", st4[:, 12:13], 0.0), reads=[bSt], writes=[bSt])
                P.op("act", _mk("activation", out=exv[:, :], in_=st4[:, 8:12], func=AF.Exp, bias=st4[:, 13:14], accum_out=st4[:, 12:13]),
                     reads=[bSt], writes=[bSt, bEx])
                P.op("dve", _mk("reciprocal", out=st4[:, 14:15], in_=st4[:, 12:13]), reads=[bSt], writes=[bSt])
                P.op("dve", _mk("tensor_scalar", out=gatew[:, c, :], in0=exv[:, :], scalar1=st4[:, 14:15], scalar2=0.0, op0=ALU.mult, op1=ALU.add), reads=[bSt, bEx], writes=[bGw])
                bk, bb = pb()
                mm(bk[:, 0:32], stri_b, Mb[:, :], True, True, [bMb] + CB, bb)
                mm(bk[:, 32:64], ones_b[:, :], Mb[:, :], True, True, [bMb] + CB, bb)
                P.op("dve", _mk("tensor_tensor", out=Tt[:, 0, :], in0=bk[:, 0:32], in1=cnt[:, :], op=ALU.add), reads=[bb, bf("cnt")], writes=[bTt])
                P.op("dve", _mk("tensor_scalar", out=Tt[:, 1, :], in0=Tt[:, 0, :], scalar1=CAP - 0.5, scalar2=1.0, op0=ALU.is_ge, op1=ALU.mult),
                     reads=[bTt], writes=[bTt])
                P.op("dve", _mk("tensor_tensor", out=Tt[:, 0, :], in0=Tt[:, 0, :], in1=ebase, op=ALU.add), reads=[bTt, bf("rep32")], writes=[bTt])
                P.op("dve", _mk("tensor_tensor", out=Tt[:, 2, :], in0=dumpT, in1=Tt[:, 0, :], op=ALU.subtract), reads=[bTt, bf("rep32")], writes=[bTt])
                P.op("dve", _mk("tensor_tensor", out=Tt[:, 2, :], in0=Tt[:, 2, :], in1=Tt[:, 1, :], op=ALU.mult), reads=[bTt], writes=[bTt])
                P.op("dve", _mk("tensor_tensor", out=Tt[:, 0, :], in0=Tt[:, 0, :], in1=Tt[:, 2, :], op=ALU.add), reads=[bTt], writes=[bTt])
                P.op("dve", _mk("tensor_tensor", out=cnt[:, :], in0=cnt[:, :], in1=bk[:, 32:64], op=ALU.add), reads=[bb, bf("cnt")], writes=[bf("cnt")])
                for k in range(4):
                    P.op("dve", _mk("tensor_tensor", out=Tt[:, 2, :], in0=oh[:, k, :], in1=Tt[:, 0, :], op=ALU.mult), reads=[bOh, bTt], writes=[bTt])
                    P.op("dve", _mk("reduce_sum", out=destf[:, k:k + 1], in_=Tt[:, 2, :], axis=AX.X), reads=[bTt], writes=[bDf])
                P.op("dve", _mk("tensor_copy", out=dest[:, c, :], in_=destf[:, :]), reads=[bDf], writes=[bDest])
                for k in range(0 if NO_SCATTER else 4):
                    P.op("pool", _mk("indirect_dma_start",
                        out=Xg[:, :], out_offset=bass.IndirectOffsetOnAxis(ap=dest2[:, c * 4 + k:c * 4 + k + 1], axis=0), in_=hbf[:, :], in_offset=None), reads=[bHbf, bDest], writes=[bXG], dma="sc", nowaw=not first_sc[0])
                    first_sc[0] = False
            P.barrier(extra=[bXt, bH, bHbf])

        aoff[0] = 0
        NE_ = 0 if STOP == 1 else NEXP
        NC3 = 0 if STOP in (1, 2) else NGROUPS * 4
        wgu = [alloc([128, 8, 2048], BF16) for _ in range(2)]
        wdn = [alloc([128, 8, 1024], BF16) for _ in range(2)]
        bdr = [alloc([128, 1024], F32) for _ in range(2)]
        xgt = [alloc([128, 1024], BF16) for _ in range(2)]
        XgT = alloc([128, 8, CAP], BF16)
        actT = alloc([128, 8, CAP], BF16)
        glu = [alloc([128, 384], F32) for _ in range(2)]
        sig = [alloc([128, 384], F32) for _ in range(2)]
        lin = [alloc([128, 384], F32) for _ in range(2)]
        Yt = [alloc([128, 1024], F32) for _ in range(2)]
        p2_end = aoff[0]
        bWgu, bWdn, bBdr = [bf("wgu0"), bf("wgu1")], [bf("wdn0"), bf("wdn1")], [bf("bdr0"), bf("bdr1")]
        bXgt = [bf("xgt0"), bf("xgt1")]
        bXgT, bActT = bf("XgT"), bf("actT")
        bGlu, bSig, bLin = [bf("glu0"), bf("glu1")], [bf("sig0"), bf("sig1")], [bf("lin0"), bf("lin1")]
        bYt = [bf("Yt0"), bf("Yt1")]

        def expert_loads(e_):
            s = e_ % 2
            for q in range(4):
                P.op("pool", _mk("dma_start",
                    out=wgu[s][:, :, q * 512:(q + 1) * 512], in_=w_gu[e_].rearrange("(kt p) c -> p kt c", p=128)[:, :, q * 512:(q + 1) * 512]),
                    writes=[bWgu[s]], dma="wgu%d" % s, nowaw=(q > 0))
            for q in range(2):
                P.op("pool", _mk("dma_start",
                    out=wdn[s][:, :, q * 512:(q + 1) * 512], in_=w_dn[e_].rearrange("(kt p) c -> p kt c", p=128)[:, :, q * 512:(q + 1) * 512]),
                    writes=[bWdn[s]], dma="wdn%d" % s, nowaw=(q > 0))
            P.op("sp", _mk("dma_start", out=bdr[s][:, :], in_=bd_rep[e_]), writes=[bBdr[s]], dma="bdr%d" % s)

        if NE_:
            expert_loads(0)
        for e_ in range(NE_):
            s = e_ % 2
            if e_ + 1 < NE_:
                expert_loads(e_ + 1)
            for stl in range(6):
                xs_ = stl % 2
                r0 = e_ * CAP + stl * 128
                P.op("sp", _mk("dma_start", out=xgt[xs_][:, :], in_=Xg[r0:r0 + 128, :]), reads=[bXG], writes=[bXgt[xs_]], dma="xgt%d" % xs_)
                bk, bb = pb()
                pbf = bk[:, :].bitcast(BF16)
                for j in range(8):
                    tr(pbf[:, j * 128:(j + 1) * 128], xgt[xs_][:, j * 128:(j + 1) * 128], ident_b, [bXgt[xs_]] + CB, bb)
                P.op("act", _mk("activation", func=AF.Copy, out=XgT[:, :, stl * 128:(stl + 1) * 128], in_=v3(pbf, 128)), reads=[bb], writes=[bXgT])
            for fi in range(8):
                for hf in range(2):
                    cs = slice(hf * 384, (hf + 1) * 384)
                    r = (fi * 2 + hf) % 2
                    bkG, bbG = pb()
                    for kt in range(8):
                        mm(bkG[:, 0:384], wgu[s][:, kt, fi * 128:(fi + 1) * 128], XgT[:, kt, cs], kt == 0, kt == 7, [bWgu[s], bXgT], bbG)
                    bkL, bbL = pb()
                    for kt in range(8):
                        mm(bkL[:, 0:384], wgu[s][:, kt, 1024 + fi * 128:1024 + (fi + 1) * 128], XgT[:, kt, cs], kt == 0, kt == 7, [bWgu[s], bXgT], bbL)
                    P.op("dve", _mk("tensor_scalar",
                        out=glu[r][:, :], in0=bkG[:, 0:384], scalar1=bgu[:, e_, fi:fi + 1], scalar2=7.0, op0=ALU.add, op1=ALU.min),
                        reads=[bbG, bf("bgu")], writes=[bGlu[r]])
                    P.op("act", _mk("activation", out=sig[r][:, :], in_=glu[r][:, :], func=AF.Sigmoid, scale=1.702), reads=[bGlu[r]], writes=[bSig[r]])
                    P.op("dve", _mk("tensor_scalar",
                        out=lin[r][:, :], in0=bkL[:, 0:384], scalar1=bgu[:, e_, 8 + fi:9 + fi], scalar2=7.0, op0=ALU.add, op1=ALU.min),
                        reads=[bbL, bf("bgu")], writes=[bLin[r]])
                    P.op("dve", _mk("tensor_scalar", out=lin[r][:, :], in0=lin[r][:, :], scalar1=-7.0, scalar2=1.0, op0=ALU.max, op1=ALU.add),
                         reads=[bLin[r]], writes=[bLin[r]])
                    P.op("pool", _mk("tensor_tensor", out=sig[r][:, :], in0=sig[r][:, :], in1=glu[r][:, :], op=ALU.mult),
                         reads=[bSig[r], bGlu[r]], writes=[bSig[r]])
                    P.op("pool", _mk("tensor_tensor", out=actT[:, fi, cs], in0=sig[r][:, :], in1=lin[r][:, :], op=ALU.mult),
                         reads=[bSig[r], bLin[r]], writes=[bActT])
            for stl in range(6):
                ys = stl % 2
                r0 = e_ * CAP + stl * 128
                for hf in range(2):
                    hs = slice(hf * 512, (hf + 1) * 512)
                    bk, bb = pb()
                    for ft in range(8):
                        mm(bk[:, :], actT[:, ft, stl * 128:(stl + 1) * 128], wdn[s][:, ft, hs], ft == 0, ft == 7, [bActT, bWdn[s]], bb)
                    P.op("dve", _mk("tensor_tensor", out=Yt[ys][:, hs], in0=bk[:, :], in1=bdr[s][:, hs], op=ALU.add),
                         reads=[bb, bBdr[s]], writes=[bYt[ys]])
                P.op("sp", _mk("dma_start", out=Yg[r0:r0 + 128, :], in_=Yt[ys][:, :]), reads=[bYt[ys]], writes=[bYG], dma="yst%d" % ys, nowaw=True)
        P.barrier(extra=bYt + bXgt)

        aoff[0] = 0
        yk = [[alloc([128, 1024], F32) for _ in range(4)] for _ in range(2)]
        h3 = [alloc([128, 1024], F32) for _ in range(2)]
        ac3 = [alloc([128, 1024], F32) for _ in range(2)]
        jk3 = alloc([128, 1024], F32)
        st3 = alloc([128, 16], F32)
        bYk = [[bf("yk%d_%d" % (r, k)) for k in range(4)] for r in range(2)]
        bH3, bAc3 = [bf("h3_0"), bf("h3_1")], [bf("ac3_0"), bf("ac3_1")]
        bJk3, bSt3 = bf("jk3"), bf("st3")
        for c in range(NC3):
            r = c % 2
            rows = slice(c * 128, (c + 1) * 128)
            for k in range(4):
                P.op("pool", _mk("indirect_dma_start",
                    out=yk[r][k][:, :], out_offset=None, in_=Yg[:, :], in_offset=bass.IndirectOffsetOnAxis(ap=dest2[:, c * 4 + k:c * 4 + k + 1], axis=0)), reads=[bYG, bDest], writes=[bYk[r][k]], dma="yk%d_%d" % (r, k))
            P.op("sp", _mk("dma_start", out=h3[r][:, :], in_=hbuf[rows, :]), reads=[bHB], writes=[bH3[r]], dma="h3_%d" % r)
            a3 = ac3[r]
            P.op("dve", _mk("tensor_scalar", out=a3[:, :], in0=yk[r][0][:, :], scalar1=gatew[:, c, 0:1], scalar2=0.0, op0=ALU.mult, op1=ALU.add),
                 reads=[bYk[r][0], bGw], writes=[bAc3[r]])
            for k in range(1, 4):
                P.op("dve", _mk("scalar_tensor_tensor",
                    out=a3[:, :], in0=yk[r][k][:, :], scalar=gatew[:, c, k:k + 1], in1=a3[:, :], op0=ALU.mult, op1=ALU.add),
                    reads=[bYk[r][k], bGw, bAc3[r]], writes=[bAc3[r]])
            P.op("dve", _mk("scalar_tensor_tensor", out=a3[:, :], in0=h3[r][:, :], scalar=ALPHA, in1=a3[:, :], op0=ALU.mult, op1=ALU.add),
                 reads=[bH3[r], bAc3[r]], writes=[bAc3[r]])
            emit_ln(P, a3, st3, jk3, bAc3[r], bSt3, bJk3, lnrep[:, 2, :], lnrep[:, 3, :], bf("lnrep"))
            P.op("sp", _mk("dma_start", out=out_d[rows, :], in_=a3[:, :]), reads=[bAc3[r]], writes=[bOUT], dma="out%d" % r, nowaw=True)
        P.op("sp", _mk("nop", ), reads=[bOUT, bHB, bXT] + sl_b, force=True)
        P.run()
    return nc


def emit_ln(P, x, st, junk, bX, bSt, bJ, g_rep, b_rep, bLn):
    jv = junk if len(junk.shape) == 2 else junk.rearrange("p a b -> p (a b)")
    P.op("dve", _mk("memset", st[:, 0:4], 0.0), reads=[bSt], writes=[bSt])
    P.op("act", _mk("activation", out=jv[:, 0:1024], in_=x[:, :], func=AF.Identity, accum_out=st[:, 0:1]), reads=[bX, bSt], writes=[bSt, bJ])
    P.op("dve", _mk("tensor_scalar", out=st[:, 1:2], in0=st[:, 0:1], scalar1=-1.0 / 1024.0, scalar2=0.0, op0=ALU.mult, op1=ALU.add),
         reads=[bSt], writes=[bSt])
    P.op("act", _mk("activation", out=jv[:, 0:1024], in_=x[:, :], func=AF.Square, bias=st[:, 1:2], accum_out=st[:, 2:3]),
         reads=[bX, bSt], writes=[bSt, bJ])
    P.op("act", _mk("activation", out=st[:, 3:4], in_=st[:, 2:3], func=AF.Ln, scale=1.0 / 1024.0, bias=1e-5), reads=[bSt], writes=[bSt])
    P.op("act", _mk("activation", out=st[:, 4:5], in_=st[:, 3:4], func=AF.Exp, scale=-0.5), reads=[bSt], writes=[bSt])
    P.op("dve", _mk("tensor_scalar", out=x[:, :], in0=x[:, :], scalar1=st[:, 1:2], scalar2=st[:, 4:5], op0=ALU.add, op1=ALU.mult),
         reads=[bX, bSt], writes=[bX])
    P.op("pool", _mk("tensor_tensor", out=x[:, :], in0=x[:, :], in1=g_rep, op=ALU.mult), reads=[bX, bLn], writes=[bX])
    P.op("pool", _mk("tensor_tensor", out=x[:, :], in0=x[:, :], in1=b_rep, op=ALU.add), reads=[bX, bLn], writes=[bX])


def _consts():
    k = np.arange(128)[:, None]
    t = np.arange(128)[None, :]
    ident = (k == t).astype(np.float32)
    tri = (k <= t).astype(np.float32)
    stri = (k < t).astype(np.float32)
    negm_cur = np.where(k <= t, 0.0, NEG).astype(np.float32)
    negm_prev = np.where(k > t, 0.0, NEG).astype(np.float32)
    return np.concatenate([ident, tri, stri, negm_cur, negm_prev], axis=1)


def _prep(inp):
    f = lambda a: np.ascontiguousarray(np.asarray(a, dtype=np.float32))
    w_in = f(inp["w_in"])[0]
    o = np.cumsum((0, 2048, 3072, 32, 1024, 256, 256, 2048))
    z, xbc, dt, q, k, v, gts = [w_in[:, o[i]:o[i + 1]] for i in range(7)]
    kdup = np.concatenate([np.concatenate([k[:, g * 64:(g + 1) * 64]] * 2, axis=1) for g in range(4)], axis=1)
    w_fm = np.concatenate([xbc, q, kdup], axis=1)
    w_vdt = np.concatenate([v, dt], axis=1)
    g0, g1 = gts[:, :1024], gts[:, 1024:]
    w_gp = np.concatenate([np.concatenate([g0[:, p * 256:(p + 1) * 256], g1[:, p * 256:(p + 1) * 256]], axis=1) for p in range(4)], axis=1)
    pl = lambda vec, nt: f(vec).reshape(nt, 128).T
    conv_w = f(inp["conv_w"])[0]
    convw_l = np.stack([pl(conv_w[j], 24) for j in range(4)], axis=2).reshape(128, 96)
    rep = lambda vec: np.broadcast_to(f(vec).reshape(1, -1), (128, f(vec).size))
    sinks = np.zeros(32, np.float32)
    sinks[:16] = f(inp["attn_sinks"])[0]
    ebase = (np.arange(32) * CAP).astype(np.float32)
    rep32 = np.concatenate([rep(inp["dt_bias"][0]), rep(inp["a_log"][0]), rep(inp["d_skip"][0]), rep(inp["b_router"][0]), rep(sinks), rep(ebase), np.broadcast_to((NEXP * CAP + np.arange(128, dtype=np.float32))[:, None], (128, 32))], axis=1)
    ln_rep = np.concatenate([rep(inp["ln1_g"][0]), rep(inp["ln1_b"][0]), rep(inp["ln2_g"][0]), rep(inp["ln2_b"][0])], axis=1)
    bgu = f(inp["b_gate_up"])[0]
    bgu_l = np.stack([pl(bgu[e], 16) for e in range(NEXP)], axis=1).reshape(128, NEXP * 16)
    bd = f(inp["b_down"])[0]
    shared = dict(
        w_fm=f(w_fm), w_z=f(z), w_vdt=f(w_vdt), w_gp=f(w_gp), w_so=f(inp["w_ssm_out"])[0], w_ao=f(inp["w_attn_out"])[0],
        w_mix=f(inp["w_mix_out"])[0], w_rt=f(inp["w_router"])[0], w_gu=f(inp["w_gate_up"])[0], w_dn=f(inp["w_down"])[0],
        bd_rep=f(np.broadcast_to(bd[:, None, :], (NEXP, 128, 1024))),
        convw_l=f(convw_l), convb_l=f(pl(inp["conv_b"][0], 24)), bgate_l=f(pl(inp["b_gates"][0], 16)),
        normw_l=f(pl(inp["ssm_norm_w"][0], 16)), bgu_l=f(bgu_l), rep32=f(rep32), ln_rep=f(ln_rep), cst=f(_consts()))
    x = f(inp["x"])
    maps = []
    for c in range(NCORES):
        xt = x[2 * c:2 * c + 2].reshape(NT, 1024)
        m = dict(shared)
        m["x_tok"] = np.ascontiguousarray(xt)
        m["xT"] = np.ascontiguousarray(xt.T)
        maps.append(m)
    return maps


def kernel(**inputs):
    maps = _prep(inputs)
    nc = build_nc()
    res = run_bass_kernel_spmd(nc, maps, core_ids=list(range(NCORES)))
    out = np.concatenate([np.asarray(r["out"]).reshape(2, 2048, 1024) for r in res.results], axis=0)
    return out.astype(np.float32)
```

```python
import contextlib
import numpy as np
import concourse.bass as bass
import concourse.mybir as mybir
from concourse.bass_utils import run_bass_kernel_spmd

F32 = mybir.dt.float32
BF16 = mybir.dt.bfloat16
I32 = mybir.dt.int32
AF = mybir.ActivationFunctionType
ALU = mybir.AluOpType
AX = mybir.AxisListType

NCORES = 8
NT = 4096
NCH = 32
NG = 8
CAP = 768
NEXP = 32
ALPHA = 2.0 ** 0.25
NEG = -30000.0
DEBUG_H = False
STOP = 0
NO_SCATTER = False
NGROUPS = NG
LIMIT = 0
DBGV = {}
MAXOPS = 0
NOBAR = False
LIMC = 4


class Buf:
    __slots__ = ("name", "w", "r", "excl")

    def __init__(self, name, excl=False):
        self.name = name
        self.w = None
        self.r = []
        self.excl = excl


class Prog:
    def __init__(self, nc, stack):
        self.nc = nc
        self.stack = stack
        self.ops = []
        self.sems = {}
        self.last = {}
        self.barbuf = {e: Buf("bar_" + e) for e in ("pe", "act", "dve", "pool")}
        self.barx = Buf("barx")
        self.pending = {}

    def op(self, eng, fn, reads=(), writes=(), dma=None, nowaw=False, force=False, final=False):
        if MAXOPS and len(self.ops) >= MAXOPS and not final:
            return len(self.ops) - 1
        i = len(self.ops)
        deps = set()
        for b in reads:
            if b.w is not None:
                deps.add(b.w)
            if b.excl:
                deps.update(r for r in b.r if self.ops[r]["eng"] != eng)
        for b in writes:
            if b.w is not None and not nowaw:
                deps.add(b.w)
            deps.update(b.r)
        for b in reads:
            b.r.append(i)
        for b in writes:
            b.w = i
            if not nowaw:
                b.r = []
        if self.pending.get(eng):
            deps.update(self.pending.pop(eng))
            force = True
        deps.discard(i)
        self.ops.append(dict(eng=eng, fn=fn, deps=deps, dma=dma, needed=False, token=None, waits=[], force=force))
        if dma is None:
            self.last[eng] = i
        return i

    def barrier(self, extra=()):
        if NOBAR:
            return
        ids = set(self.last.values())
        for b in extra:
            if b.w is not None:
                ids.add(b.w)
            ids.update(b.r)
        for e in ("pe", "act", "dve", "pool", "sp"):
            self.pending.setdefault(e, set()).update(ids)

    def _skip(self, op, D):
        return (D["dma"] is None and op["dma"] is None and D["eng"] == "pe" and op["eng"] == "pe"
                and not op["force"] and not D["force"])

    def finish(self):
        lastd = {}
        for i, op in enumerate(self.ops):
            if op["dma"] is not None:
                lastd[op["dma"]] = i
        lastv = list(self.last.values())
        i = self.op("sp", lambda en: en.nop(), force=True, final=True)
        self.ops[i]["deps"].update(lastd.values())
        self.ops[i]["deps"].update(lastv)
        self.ops[i]["deps"].discard(i)

    def run(self):
        self.finish()
        ops = self.ops
        for op in ops:
            for d in op["deps"]:
                if not self._skip(op, ops[d]):
                    ops[d]["needed"] = True
        cnt = {}
        for op in ops:
            if op["dma"] is not None:
                k = ("d", op["dma"])
                cnt[k] = cnt.get(k, 0) + 16
                op["token"] = (k, cnt[k])
            elif op["needed"]:
                k = ("e", op["eng"])
                cnt[k] = cnt.get(k, 0) + 1
                op["token"] = (k, cnt[k])
        waited = {}
        for op in ops:
            ws = {}
            for d in op["deps"]:
                D = ops[d]
                if self._skip(op, D):
                    continue
                k, v = D["token"]
                ws[k] = max(ws.get(k, 0), v)
            wd = waited.setdefault(op["eng"], {})
            op["waits"] = [(k, v) for k, v in ws.items() if wd.get(k, 0) < v]
            for k, v in op["waits"]:
                wd[k] = v
        for op in ops:
            if op["token"] and op["token"][0] not in self.sems:
                k = op["token"][0]
                self.sems[k] = self.stack.enter_context(self.nc.semaphore("s_%s_%s" % k))
        engs = {"pe": "tensor", "act": "scalar", "dve": "vector", "pool": "gpsimd", "sp": "sync"}
        with self.nc.Block() as block:
            for en, attr in engs.items():
                mine = [op for op in ops if op["eng"] == en]
                if not mine:
                    continue

                def body(e, mine=mine):
                    for op in mine:
                        for k, v in op["waits"]:
                            e.wait_ge(self.sems[k], v)
                        try:
                            ins = op["fn"](e)
                        except Exception:
                            print("FAILED OP eng", op["eng"], "dma", op["dma"], "nsems", len(self.sems), "free", self.nc.free_len())
                            raise
                        if op["token"]:
                            ins.then_inc(self.sems[op["token"][0]], 16 if op["dma"] is not None else 1)

                getattr(block, attr)(body)


def _mk(meth, *a, **k):
    return lambda e: getattr(e, meth)(*a, **k)


def v3(ap, b):
    return ap.rearrange("p (a b) -> p a b", b=b)


def bc(ap, m):
    p, n = ap.shape
    return ap.unsqueeze(2).to_broadcast([p, n, m])


def build_nc():
    nc = bass.Bass("TRN2", target_bir_lowering=False)
    din = lambda name, shape, dt=F32: nc.dram_tensor(name, list(shape), dt, kind="ExternalInput").ap()
    x_tok = din("x_tok", [NT, 1024])
    xT_d = din("xT", [1024, NT])
    w_fm = din("w_fm", [1024, 4608])
    w_z = din("w_z", [1024, 2048])
    w_vdt = din("w_vdt", [1024, 288])
    w_gp = din("w_gp", [1024, 2048])
    w_so = din("w_so", [2048, 1024])
    w_ao = din("w_ao", [1024, 1024])
    w_mix = din("w_mix", [1024, 1024])
    w_rt = din("w_rt", [1024, 32])
    NXD = 1 if STOP == 1 else NEXP
    w_gu = din("w_gu", [NXD, 1024, 2048])
    w_dn = din("w_dn", [NXD, 1024, 1024])
    bd_rep = din("bd_rep", [NXD, 128, 1024])
    convw_d = din("convw_l", [128, 24 * 4])
    convb_d = din("convb_l", [128, 24])
    bgate_d = din("bgate_l", [128, 16])
    normw_d = din("normw_l", [128, 16])
    bgu_d = din("bgu_l", [128, NEXP * 16])
    rep32_d = din("rep32", [128, 7 * 32])
    lnrep_d = din("ln_rep", [128, 4 * 1024])
    cst_d = din("cst", [128, 5 * 128])
    out_d = nc.dram_tensor("out", [NT, 1024], F32, kind="ExternalOutput").ap()
    hkind = "ExternalOutput" if DEBUG_H else "Internal"
    hbuf = nc.dram_tensor("hbuf", [NT, 1024], F32, kind=hkind).ap()
    Xg = nc.dram_tensor("Xg", [NEXP * CAP + 128, 1024], BF16, kind="Internal").ap()
    Yg = nc.dram_tensor("Yg", [NEXP * CAP + 128, 1024], F32, kind="Internal").ap()

    with contextlib.ExitStack() as st:
        P = Prog(nc, st)
        sbt = lambda name, shape, dt: st.enter_context(nc.sbuf_tensor("sb_" + name, list(shape), dt))
        banks = [st.enter_context(nc.psum_tensor("ps%d" % i, [128, 512], F32)) for i in range(8)]
        bbufs = [Buf("ps%d" % i, excl=True) for i in range(8)]
        bstate = [0]

        def pb():
            i = bstate[0] % 8
            bstate[0] += 1
            return banks[i], bbufs[i]

        cst = sbt("cst", [128, 640], F32)
        ident_f = cst[:, 0:128]
        tri = cst[:, 128:256]
        cstb = sbt("cstb", [128, 640], BF16)
        ident_b = cstb[:, 0:128]
        stri_b = cstb[:, 256:384]
        ones_f = sbt("ones_f", [128, 128], F32)
        ones_b = sbt("ones_b", [128, 128], BF16)
        negm = sbt("negm", [128, 2, 512], BF16)
        convw = sbt("convw", [128, 24, 4], F32)
        convb = sbt("convb", [128, 24], F32)
        bgate = sbt("bgate", [128, 16], F32)
        normw = sbt("normw", [128, 16], F32)
        bgu = sbt("bgu", [128, NEXP, 16], F32)
        rep32 = sbt("rep32", [128, 7, 32], F32)
        lnrep = sbt("lnrep", [128, 4, 1024], F32)
        A_rep = sbt("A_rep", [128, 32], F32)
        esink = sbt("esink", [128, 16], F32)
        cnt = sbt("cnt", [128, 32], F32)
        dest2 = sbt("dest", [128, NCH * 4], I32)
        dest = v3(dest2[:, :], 4)
        gatew = sbt("gatew", [128, NCH, 4], F32)
        wrt = sbt("wrt", [128, 8, 32], F32)
        wvdt = sbt("wvdt", [128, 8, 288], BF16)
        B = {}

        def bf(n):
            if n not in B:
                B[n] = Buf(n)
            return B[n]

        ld = lambda dst, src, name: P.op("sp", _mk("dma_start", out=dst, in_=src), writes=[bf(name)], dma="c_" + name)
        ld(cst[:, :], cst_d, "cst")
        ld(convw[:, :, :], v3(convw_d, 4), "convw")
        ld(convb[:, :], convb_d, "convb")
        ld(bgate[:, :], bgate_d, "bgate")
        ld(normw[:, :], normw_d, "normw")
        ld(bgu[:, :, :], v3(bgu_d, 16), "bgu")
        ld(rep32[:, :, :], v3(rep32_d, 32), "rep32")
        ld(lnrep[:, :, :], v3(lnrep_d, 1024), "lnrep")
        ld(wrt[:, :, :], w_rt.rearrange("(kt p) c -> p kt c", p=128), "wrt")
        P.op("pool", _mk("dma_start", out=wvdt[:, :, :], in_=w_vdt.rearrange("(kt p) c -> p kt c", p=128)),
             writes=[bf("wvdt")], dma="c_wvdt")
        P.op("dve", _mk("tensor_copy", out=cstb[:, :], in_=cst[:, :]), reads=[bf("cst")], writes=[bf("cstb")])
        P.op("dve", _mk("memset", ones_f[:, :], 1.0), writes=[bf("ones_f")])
        P.op("dve", _mk("memset", ones_b[:, :], 1.0), writes=[bf("ones_b")])
        P.op("dve", _mk("memset", cnt[:, :], 0.0), writes=[bf("cnt")])
        for kt_ in range(2):
            for r_ in range(4):
                c0_ = 512 if kt_ == 0 else 384
                P.op("dve", _mk("tensor_copy",
                    out=negm[:, kt_, r_ * 128:(r_ + 1) * 128], in_=cst[:, c0_:c0_ + 128]),
                    reads=[bf("cst")], writes=[bf("negm")])
        P.op("act", _mk("activation", out=A_rep[:, :], in_=rep32[:, 1, :], func=AF.Exp), reads=[bf("rep32")], writes=[bf("A_rep")])
        P.op("dve", _mk("tensor_scalar", out=A_rep[:, :], in0=A_rep[:, :], scalar1=-1.0, scalar2=0.0, op0=ALU.mult, op1=ALU.add),
             reads=[bf("A_rep")], writes=[bf("A_rep")])
        P.op("act", _mk("activation", out=esink[:, :], in_=rep32[:, 4, 0:16], func=AF.Exp), reads=[bf("rep32")], writes=[bf("esink")])
        dtb_rep = rep32[:, 0, :]
        D_rep = rep32[:, 2, :]
        brt_rep = rep32[:, 3, :]
        ebase = rep32[:, 5, :]
        dumpT = rep32[:, 6, :]
        CB = [bf("cst"), bf("cstb"), bf("negm"), bf("ones_f"), bf("ones_b")]

        ARW = 43200
        arena = sbt("arena", [128, ARW], F32)
        aoff = [0]

        def alloc(shape, dt, name=None):
            if name:
                DBGV[name] = (aoff[0], tuple(shape), dt)
            n = int(np.prod(shape[1:]))
            words = n if dt in (F32, I32) else (n + 1) // 2
            words = (words + 7) // 8 * 8
            v = arena[0:shape[0], aoff[0]:aoff[0] + words]
            aoff[0] += words
            assert aoff[0] <= ARW, aoff[0]
            if dt != F32:
                v = v.bitcast(dt)
            v = v[:, 0:n]
            if len(shape) == 3:
                v = v3(v, shape[2])
            elif len(shape) == 4:
                v = v.rearrange("p (a b c) -> p a b c", b=shape[2], c=shape[3])
            return v

        xTb = alloc([128, 8, 512], BF16, "xTb")
        slabs = [alloc([128, 4096], BF16) for _ in range(3)]
        sl_b = [Buf("slab%d" % i) for i in range(3)]
        slst = [0]
        xbc = alloc([128, 24, 512], BF16, "xbc")
        QT = alloc([128, 2, 8, 512], BF16, "QT")
        KT = alloc([128, 4, 640], BF16, "KT")
        Vaug = alloc([128, 2, 4, 68], BF16, "Vaug")
        sz = alloc([128, 4, 2048], BF16, "sz")
        hist = alloc([128, 24, 3], F32)
        S = alloc([128, 2048], F32, "S")
        Sbf = alloc([128, 2048], BF16)
        region0 = aoff[0]
        U = [alloc([128, 515], F32) for _ in range(2)]
        acc = [alloc([128, 512], F32) for _ in range(2)]
        aoff[0] = region0
        sm = alloc([128, 12, 32], F32, "sm")
        xtok = alloc([128, 2048], BF16, "xtok")
        xs = alloc([128, 2048], BF16, "xs")
        xsw = alloc([128, 2048], BF16)
        Btok = alloc([128, 4, 128], BF16, "Btok")
        cbT = alloc([128, 4, 128], F32, "cbT")
        LT = [alloc([128, 512], F32) for _ in range(2)]
        MT = [alloc([128, 512], BF16) for _ in range(4)]
        t1 = [alloc([128, 512], F32) for _ in range(2)]
        t2 = [alloc([128, 512], F32) for _ in range(2)]
        yz = alloc([128, 2048], F32, "yz")
        junk = alloc([128, 1024], F32)
        yn = alloc([128, 2048], BF16, "yn")
        ss = alloc([128, 8], F32)
        PT = [alloc([128, 2, 512], BF16) for _ in range(2)]
        atok = alloc([128, 1024], BF16, "atok")
        rec = alloc([128, 8], F32)
        regionC_end = aoff[0]
        aoff[0] = region0
        mergedT = alloc([128, 8, 512], BF16, "mergedT")
        g1m = [alloc([128, 2, 512], F32) for _ in range(2)]
        g0t = [alloc([128, 2, 512], F32) for _ in range(2)]
        xt32 = alloc([128, 1024], F32)
        hh = alloc([128, 1024], F32, "hh")
        hbf = alloc([128, 1024], BF16)
        hT = alloc([128, 8, 128], F32)
        st4 = alloc([128, 16], F32)
        lg = alloc([128, 32], F32)
        oh = alloc([128, 4, 32], F32)
        Mm = alloc([128, 32], F32)
        Mb = alloc([128, 32], BF16)
        Tt = alloc([128, 3, 32], F32)
        destf = alloc([128, 4], F32)
        exv = alloc([128, 4], F32)
        regionDE_end = aoff[0]
        p1_end = max(regionC_end, regionDE_end)
        ynT = xbc[:, 0:16, :]
        aT = xbc[:, 16:24, :]

        plan = []
        issued = [0]

        NSL = 27
        wscr = nc.dram_tensor("wscr", [NSL, 128, 4096], BF16, kind="Internal").ap()
        bScr = [Buf("wscr%d" % k_) for k_ in range(NSL)]

        def slab_issue_upto(n):
            while issued[0] < min(n, len(plan)):
                i = issued[0]
                src_ap, (a, b) = plan[i]
                k_ = i % NSL
                sl2 = slabs[i % 3][:, 0:a * b]
                if i < NSL:
                    view = v3(sl2, b)
                    P.op("pool", _mk("dma_start", out=view, in_=src_ap), writes=[sl_b[i % 3]], dma="slab%d" % (i % 3))
                    P.op("sp", _mk("dma_start", out=wscr[k_][:, 0:a * b], in_=sl2), reads=[sl_b[i % 3]], writes=[bScr[k_]],
                         dma="wst%d" % (i % 3))
                else:
                    P.op("sp", _mk("dma_start", out=sl2, in_=wscr[k_][:, 0:a * b]), reads=[bScr[k_]], writes=[sl_b[i % 3]],
                         dma="slabh%d" % (i % 3))
                issued[0] += 1

        def slab_load(src_ap, shape3):
            i = slst[0]
            slst[0] += 1
            assert plan[i][1] == shape3, (i, plan[i][1], shape3)
            slab_issue_upto(i + 2)
            a, b = shape3
            return v3(slabs[i % 3][:, 0:a * b], b), sl_b[i % 3]

        def mm(out, lhsT, rhs, start, stop, reads, wbuf):
            P.op("pe", _mk("matmul", out, lhsT=lhsT, rhs=rhs, start=start, stop=stop), reads=reads, writes=[wbuf])

        def tr(out, in_, ident, reads, wbuf):
            P.op("pe", _mk("transpose", out=out, in_=in_, identity=ident), reads=reads, writes=[wbuf])

        wfm_v = w_fm.rearrange("(kt p) c -> p kt c", p=128)
        wz_v = w_z.rearrange("(kt p) c -> p kt c", p=128)
        wgp_v = w_gp.rearrange("(kt p) c -> p kt c", p=128)
        wso_v = w_so.rearrange("(kt p) c -> p kt c", p=128)
        wao_v = w_ao.rearrange("(kt p) c -> p kt c", p=128)
        wmix_v = w_mix.rearrange("(kt p) c -> p kt c", p=128)
        xT_v = xT_d.rearrange("(kt p) t -> p kt t", p=128)
        for g_ in range(NGROUPS):
            for sl in range(9):
                plan.append((wfm_v[:, :, sl * 512:(sl + 1) * 512], (8, 512)))
            for zs in range(4):
                plan.append((wz_v[:, :, zs * 512:(zs + 1) * 512], (8, 512)))
            for pi in range(4):
                plan.append((wgp_v[:, :, pi * 512:(pi + 1) * 512], (8, 512)))
                plan.append((wao_v[:, :, pi * 256:(pi + 1) * 256], (8, 256)))
                plan.append((wso_v[:, :, pi * 256:(pi + 1) * 256], (16, 256)))
            for hf in range(2):
                plan.append((wmix_v[:, :, hf * 512:(hf + 1) * 512], (8, 512)))

        bXT = bf("xT")
        bXx = [bf("xbc_x%d" % i) for i in range(4)]
        bXbc = [bf("xbc_bc%d" % i) for i in range(4)]
        bQT, bKT, bV = bf("QT"), bf("KT"), [bf("V0"), bf("V1")]
        bSZ = [bf("sz%d" % i) for i in range(4)]
        bHist = [bf("hist%d" % i) for i in range(24)]
        bS = [bf("S%d" % i) for i in range(4)]
        bSb = [bf("Sb%d" % i) for i in range(4)]
        bU = [bf("U0"), bf("U1")]
        bAcc = [bf("acc0"), bf("acc1")]
        PRM = [bf("convw"), bf("convb")]
        bOUT = bf("OUT")
        bHB = bf("hbufd")
        bXG = bf("Xgd")
        bYG = bf("Ygd")
        bDest = bf("dest")
        bGw = bf("gatew")
        P.op("dve", _mk("memset", Vaug[:, :, :, :], 1.0), writes=bV)
        P.op("dve", _mk("memset", QT[:, :, :, :], 0.0), writes=[bQT])
        ztile = sbt("ztile", [128, 1024], BF16)
        P.op("dve", _mk("memset", ztile[:, :], 0.0), writes=[bf("ztile")])
        for r0_ in range(0, NEXP * CAP + 128, 128):
            P.op("sp", _mk("dma_start", out=Xg[r0_:r0_ + 128, :], in_=ztile[:, :]), reads=[bf("ztile")], writes=[bXG], dma="xgz", nowaw=True)
        first_sc = [True]

        for g in range(NGROUPS):
            gi = g % 4
            first_grp = gi == 0
            tok0 = g * 512
            P.op("pool", _mk("dma_start", out=xTb[:, :, :], in_=xT_v[:, :, tok0:tok0 + 512]),
                 writes=[bXT], dma="xT")
            if first_grp:
                P.op("dve", _mk("memset", S[:, :], 0.0), writes=bS)
                P.op("dve", _mk("memset", Sbf[:, :], 0.0), writes=bSb)
            else:
                P.op("pool", _mk("tensor_copy", out=KT[:, :, 0:128], in_=KT[:, :, 512:640]), reads=[bKT], writes=[bKT])
            for sl in range(9):
                wv, wb_ = slab_load(wfm_v[:, :, sl * 512:(sl + 1) * 512], (8, 512))
                for t4 in range(4):
                    ti = sl * 4 + t4
                    bk, bb = pb()
                    for kt in range(8):
                        mm(bk[:, :], wv[:, kt, t4 * 128:(t4 + 1) * 128], xTb[:, kt, :], kt == 0, kt == 7, [wb_, bXT], bb)
                    if ti < 24:
                        u = ti % 2
                        Uu, au = U[u], acc[u]
                        P.op("dve", _mk("tensor_copy", out=Uu[:, 3:515], in_=bk[:, :]), reads=[bb], writes=[bU[u]])
                        if first_grp:
                            P.op("pool", _mk("memset", Uu[:, 0:3], 0.0), writes=[bU[u]])
                        else:
                            P.op("pool", _mk("tensor_copy", out=Uu[:, 0:3], in_=hist[:, ti, :]),
                                 reads=[bHist[ti]], writes=[bU[u]])
                        P.op("dve", _mk("tensor_scalar",
                            out=au[:, :], in0=bk[:, :], scalar1=convw[:, ti, 3:4], scalar2=convb[:, ti:ti + 1], op0=ALU.mult, op1=ALU.add),
                            reads=[bb] + PRM, writes=[bAcc[u]])
                        for j in range(3):
                            P.op("dve", _mk("scalar_tensor_tensor",
                                out=au[:, :], in0=Uu[:, j:j + 512], scalar=convw[:, ti, j:j + 1], in1=au[:, :],
                                op0=ALU.mult, op1=ALU.add), reads=[bU[u], bAcc[u]] + PRM, writes=[bAcc[u]])
                        wl = bXx if ti < 16 else bXbc
                        P.op("act", _mk("activation", out=xbc[:, ti, :], in_=au[:, :], func=AF.Silu),
                             reads=[bAcc[u]], writes=wl)
                        P.op("pool", _mk("tensor_copy", out=hist[:, ti, :], in_=Uu[:, 512:515]),
                             reads=[bU[u]], writes=[bHist[ti]])
                    elif ti < 32:
                        P.op("act", _mk("activation", func=AF.Copy, out=QT[0:64, 0, ti - 24, :], in_=bk[0:64, :]), reads=[bb], writes=[bQT])
                        P.op("act", _mk("activation", func=AF.Copy, out=QT[64:128, 1, ti - 24, :], in_=bk[64:128, :]), reads=[bb], writes=[bQT])
                    else:
                        P.op("dve", _mk("tensor_copy", out=KT[:, ti - 32, 128:640], in_=bk[:, :]), reads=[bb], writes=[bKT])
            for zs in range(4):
                wv, wb_ = slab_load(wz_v[:, :, zs * 512:(zs + 1) * 512], (8, 512))
                for ch in range(4):
                    bk, bb = pb()
                    for kt in range(8):
                        mm(bk[:, :], xTb[:, kt, ch * 128:(ch + 1) * 128], wv[:, kt, :], kt == 0, kt == 7, [wb_, bXT], bb)
                    P.op("act", _mk("activation",
                        out=sz[:, ch, zs * 512:(zs + 1) * 512], in_=bk[:, :], func=AF.Silu), reads=[bb], writes=[bSZ[ch]])
            P.barrier()
            if LIMIT == 1:
                break
            for ch in range(LIMC if LIMIT == 2 else 4):
                c = g * 4 + ch
                ci = c % 16
                cols = slice(ch * 128, (ch + 1) * 128)
                cur, prv = c % 2, (c + 1) % 2
                tS, tm_, el, dtv, av, nac, ea, dd, dte, cd = [sm[:, i, :] for i in range(10)]
                bsm = bf("sm")
                bk, bb = pb()
                for kt in range(8):
                    mm(bk[:, 0:288], xTb[:, kt, cols], wvdt[:, kt, :], kt == 0, kt == 7, [bXT, bf("wvdt")], bb)
                P.op("dve", _mk("tensor_copy", out=Vaug[:, cur, :, 0:64], in_=v3(bk[:, 0:256], 64)), reads=[bb], writes=[bV[cur]])
                P.op("dve", _mk("tensor_tensor", out=tS, in0=bk[:, 256:288], in1=dtb_rep, op=ALU.add), reads=[bb, bf("rep32")], writes=[bsm])
                P.op("dve", _mk("tensor_scalar_min", out=tm_, in0=tS, scalar1=30.0), reads=[bsm], writes=[bsm])
                P.op("act", _mk("activation", out=el, in_=tm_, func=AF.Exp), reads=[bsm], writes=[bsm])
                P.op("act", _mk("activation", out=el, in_=el, func=AF.Ln, bias=1.0), reads=[bsm], writes=[bsm])
                P.op("dve", _mk("tensor_max", out=dtv, in0=tS, in1=el), reads=[bsm], writes=[bsm])
                P.op("dve", _mk("tensor_tensor", out=av, in0=dtv, in1=A_rep[:, :], op=ALU.mult), reads=[bsm, bf("A_rep")], writes=[bsm])
                bk2, bb2 = pb()
                mm(bk2[:, 0:32], tri, av, True, True, [bsm] + CB, bb2)
                mm(bk2[:, 32:64], ones_f[:, :], av, True, True, [bsm] + CB, bb2)
                P.op("dve", _mk("tensor_scalar", out=nac, in0=bk2[:, 0:32], scalar1=-1.0, scalar2=0.0, op0=ALU.mult, op1=ALU.add),
                     reads=[bb2], writes=[bsm])
                P.op("act", _mk("activation", out=ea, in_=bk2[:, 0:32], func=AF.Exp), reads=[bb2], writes=[bsm])
                P.op("dve", _mk("tensor_tensor", out=dd, in0=bk2[:, 32:64], in1=nac, op=ALU.add), reads=[bb2, bsm], writes=[bsm])
                P.op("act", _mk("activation", out=dte, in_=dd, func=AF.Exp), reads=[bsm], writes=[bsm])
                P.op("act", _mk("activation", out=cd, in_=bk2[:, 32:64], func=AF.Exp), reads=[bb2], writes=[bsm])
                bXs, bXtok, bXsw, bBtok = bf("xs"), bf("xtok"), bf("xsw"), bf("Btok")
                for bi in range(2):
                    bk, bb = pb()
                    pbf = bk[:, :].bitcast(BF16)
                    for j in range(8):
                        tr(pbf[:, j * 128:(j + 1) * 128], xbc[:, bi * 8 + j, cols], ident_b, [bXx[ch]] + CB, bb)
                    P.op("act", _mk("activation", func=AF.Copy, out=xtok[:, bi * 1024:(bi + 1) * 1024], in_=pbf), reads=[bb], writes=[bXtok])
                    P.op("dve", _mk("tensor_tensor",
                        out=v3(xs[:, bi * 1024:(bi + 1) * 1024], 64), in0=v3(pbf, 64), in1=bc(dtv[:, bi * 16:(bi + 1) * 16], 64), op=ALU.mult),
                        reads=[bb, bsm, bXtok], writes=[bXs])
                P.op("dve", _mk("tensor_tensor", out=v3(xsw[:, :], 64), in0=v3(xs[:, :], 64), in1=bc(dte, 64), op=ALU.mult),
                     reads=[bXs, bsm], writes=[bXsw])
                bk, bb = pb()
                pbf = bk[:, :].bitcast(BF16)
                for gg in range(4):
                    tr(pbf[:, gg * 128:(gg + 1) * 128], xbc[:, 16 + gg, cols], ident_b, [bXbc[ch]] + CB, bb)
                P.op("act", _mk("activation", func=AF.Copy, out=Btok[:, :, :], in_=v3(pbf[:, 0:512], 128)), reads=[bb], writes=[bBtok])
                bk, bb = pb()
                for gg in range(4):
                    mm(bk[:, gg * 128:(gg + 1) * 128], xbc[:, 16 + gg, cols], xbc[:, 20 + gg, cols], True, True, [bXbc[ch]], bb)
                bcb = bf("cbT")
                P.op("act", _mk("activation", func=AF.Copy, out=cbT[:, :, :], in_=v3(bk[:, :], 128)), reads=[bb], writes=[bcb])
                byz = bf("yz")
                bss = bf("ss")
                P.op("dve", _mk("memset", ss[:, :], 0.0), writes=[bss])
                for gg in range(4):
                    bkA, bbA = pb()
                    for hf in range(2):
                        bkR, bbR = pb()
                        li = (gg * 2 + hf) % 2
                        mi = (gg * 2 + hf) % 4
                        bLT, bMT = bf("LT%d" % li), bf("MT%d" % mi)
                        for j in range(4):
                            h = 8 * gg + 4 * hf + j
                            mm(bkR[:, j * 128:(j + 1) * 128], ident_b, negm[:, 1, 0:128], True, False, CB, bbR)
                            mm(bkR[:, j * 128:(j + 1) * 128], av[:, h:h + 1].to_broadcast([128, 128]), tri, False, True, [bsm] + CB, bbR)
                        for j in range(4):
                            h = 8 * gg + 4 * hf + j
                            P.op("act", _mk("activation",
                                out=LT[li][:, j * 128:(j + 1) * 128], in_=bkR[:, j * 128:(j + 1) * 128], func=AF.Exp, bias=nac[:, h:h + 1]),
                                reads=[bbR, bsm], writes=[bLT])
                        P.op("dve", _mk("tensor_tensor",
                            out=v3(MT[mi][:, :], 128), in0=v3(LT[li][:, :], 128), in1=cbT[:, gg:gg + 1, :].to_broadcast([128, 4, 128]), op=ALU.mult),
                            reads=[bLT, bcb], writes=[bMT])
                        for j in range(4):
                            h = 8 * gg + 4 * hf + j
                            mm(bkA[:, (4 * hf + j) * 64:(4 * hf + j + 1) * 64], MT[mi][:, j * 128:(j + 1) * 128], xs[:, h * 64:(h + 1) * 64],
                               True, True, [bMT, bXs], bbA)
                    gs = slice(gg * 512, (gg + 1) * 512)
                    bkB, bbB = pb()
                    mm(bkB[:, :], xbc[:, 20 + gg, cols], Sbf[:, gs], True, True, [bXbc[ch], bSb[gg]], bbB)
                    bkC, bbC = pb()
                    mm(bkC[:, :], Btok[:, gg, :], xsw[:, gs], True, True, [bBtok, bXsw], bbC)
                    ti_ = gg % 2
                    bt1, bt2 = bf("t1_%d" % ti_), bf("t2_%d" % ti_)
                    P.op("dve", _mk("tensor_tensor",
                        out=v3(t1[ti_][:, :], 64), in0=v3(bkB[:, :], 64), in1=bc(ea[:, 8 * gg:8 * gg + 8], 64), op=ALU.mult),
                        reads=[bbB, bsm], writes=[bt1])
                    P.op("pool", _mk("tensor_tensor",
                        out=v3(t2[ti_][:, :], 64), in0=v3(xtok[:, gs], 64), in1=bc(D_rep[:, 8 * gg:8 * gg + 8], 64), op=ALU.mult),
                        reads=[bXtok, bf("rep32")], writes=[bt2])
                    P.op("pool", _mk("tensor_tensor", out=t1[ti_][:, :], in0=t1[ti_][:, :], in1=t2[ti_][:, :], op=ALU.add),
                         reads=[bt1, bt2], writes=[bt1])
                    P.op("dve", _mk("tensor_tensor", out=yz[:, gs], in0=bkA[:, :], in1=t1[ti_][:, :], op=ALU.add),
                         reads=[bbA, bt1], writes=[byz])
                    P.op("dve", _mk("tensor_tensor", out=yz[:, gs], in0=yz[:, gs], in1=sz[:, ch, gs], op=ALU.mult),
                         reads=[byz, bSZ[ch]], writes=[byz])
                    P.op("act", _mk("activation", out=junk[:, 0:512], in_=yz[:, gs], func=AF.Square, accum_out=ss[:, gg:gg + 1]),
                         reads=[byz, bss], writes=[bss, bf("junk")])
                    P.op("pool", _mk("tensor_tensor",
                        out=v3(S[:, gs], 64), in0=v3(S[:, gs], 64), in1=bc(cd[:, 8 * gg:8 * gg + 8], 64), op=ALU.mult),
                        reads=[bS[gg], bsm], writes=[bS[gg]])
                    P.op("dve", _mk("tensor_tensor", out=S[:, gs], in0=S[:, gs], in1=bkC[:, :], op=ALU.add),
                         reads=[bS[gg], bbC], writes=[bS[gg]])
                    P.op("act", _mk("activation", func=AF.Copy, out=Sbf[:, gs], in_=S[:, gs]), reads=[bS[gg]], writes=[bSb[gg]])
                P.op("dve", _mk("tensor_scalar", out=ss[:, 4:8], in0=ss[:, 0:4], scalar1=1.0 / 512.0, scalar2=1e-5, op0=ALU.mult, op1=ALU.add),
                     reads=[bss], writes=[bss])
                P.op("act", _mk("activation", out=ss[:, 4:8], in_=ss[:, 4:8], func=AF.Ln), reads=[bss], writes=[bss])
                P.op("act", _mk("activation", out=ss[:, 4:8], in_=ss[:, 4:8], func=AF.Exp, scale=-0.5), reads=[bss], writes=[bss])
                byn = bf("yn")
                for gg in range(4):
                    gs = slice(gg * 512, (gg + 1) * 512)
                    P.op("dve", _mk("tensor_scalar", out=yn[:, gs], in0=yz[:, gs], scalar1=ss[:, 4 + gg:5 + gg], scalar2=0.0, op0=ALU.mult, op1=ALU.add),
                         reads=[byz, bss], writes=[byn])
                for bi in range(2):
                    bk, bb = pb()
                    pbf = bk[:, :].bitcast(BF16)
                    for j in range(8):
                        tr(pbf[:, j * 128:(j + 1) * 128], yn[:, (bi * 8 + j) * 128:(bi * 8 + j + 1) * 128], ident_b, [byn] + CB, bb)
                    P.op("dve", _mk("tensor_tensor",
                        out=ynT[:, bi * 8:(bi + 1) * 8, cols], in0=v3(pbf, 128), in1=bc(normw[:, bi * 8:(bi + 1) * 8], 128), op=ALU.mult),
                        reads=[bb, bf("normw")], writes=[bXx[ch]])
                kts = [1] if ci == 0 else [0, 1]
                bat = bf("atok")
                brec = bf("rec")
                for gg in range(4):
                    pi_ = gg % 2
                    bPT = bf("PT%d" % pi_)
                    for kt in kts:
                        kcols = slice((ch + kt) * 128, (ch + kt + 1) * 128)
                        bk, bb = pb()
                        for j in range(4):
                            js = slice(j * 128, (j + 1) * 128)
                            mm(bk[:, js], ident_b, negm[:, kt, 0:128], True, False, CB, bb)
                            mm(bk[:, js], KT[:, gg, kcols], QT[:, j % 2, 2 * gg + j // 2, cols], False, True, [bKT, bQT], bb)
                        P.op("act", _mk("activation", out=PT[pi_][:, kt, :], in_=bk[:, :], func=AF.Exp, scale=0.125),
                             reads=[bb], writes=[bPT])
                    bkO, bbO = pb()
                    for jj in range(4):
                        j = jj
                        for kt in kts:
                            blk = cur if kt == 1 else prv
                            mm(bkO[:, j * 65:(j + 1) * 65], PT[pi_][:, kt, jj * 128:(jj + 1) * 128], Vaug[:, blk, gg, 0:65],
                               kt == kts[0], kt == kts[-1], [bPT, bV[blk]], bbO)
                    O3 = v3(bkO[:, 0:260], 65)
                    P.op("dve", _mk("tensor_tensor", out=rec[:, 0:4], in0=O3[:, :, 64], in1=esink[:, 4 * gg:4 * gg + 4], op=ALU.add),
                         reads=[bbO, bf("esink")], writes=[brec])
                    P.op("dve", _mk("reciprocal", out=rec[:, 4:8], in_=rec[:, 0:4]), reads=[brec], writes=[brec])
                    P.op("dve", _mk("tensor_tensor",
                        out=v3(atok[:, gg * 256:(gg + 1) * 256], 64), in0=O3[:, :, 0:64], in1=bc(rec[:, 4:8], 64), op=ALU.mult),
                        reads=[bbO, brec], writes=[bat])
                bk, bb = pb()
                pbf = bk[:, :].bitcast(BF16)
                for j in range(8):
                    tr(pbf[:, j * 128:(j + 1) * 128], atok[:, j * 128:(j + 1) * 128], ident_b, [bat] + CB, bb)
                P.op("act", _mk("activation", func=AF.Copy, out=aT[:, :, cols], in_=v3(pbf, 128)), reads=[bb], writes=[bXbc[ch]])
            P.barrier()
            if LIMIT == 2:
                break
            bMg = bf("mergedT")
            for pi in range(4):
                ri = pi % 2
                bG1, bG0 = bf("g1m%d" % ri), bf("g0t%d" % ri)
                wv, wb_ = slab_load(wgp_v[:, :, pi * 512:(pi + 1) * 512], (8, 512))
                gb = []
                for q4 in range(4):
                    bk, bb = pb()
                    for kt in range(8):
                        mm(bk[:, :], wv[:, kt, q4 * 128:(q4 + 1) * 128], xTb[:, kt, :], kt == 0, kt == 7, [wb_, bXT], bb)
                    i2 = q4 % 2
                    if q4 < 2:
                        P.op("act", _mk("activation",
                            out=g0t[ri][:, i2, :], in_=bk[:, :], func=AF.Sigmoid, bias=bgate[:, 2 * pi + i2:2 * pi + i2 + 1]),
                            reads=[bb, bf("bgate")], writes=[bG0])
                    else:
                        P.op("act", _mk("activation",
                            out=g1m[ri][:, i2, :], in_=bk[:, :], func=AF.Sigmoid, bias=bgate[:, 8 + 2 * pi + i2:8 + 2 * pi + i2 + 1]),
                            reads=[bb, bf("bgate")], writes=[bG1])
                wv, wb_ = slab_load(wao_v[:, :, pi * 256:(pi + 1) * 256], (8, 256))
                for i2 in range(2):
                    bk, bb = pb()
                    for kt in range(8):
                        mm(bk[:, :], wv[:, kt, i2 * 128:(i2 + 1) * 128], aT[:, kt, :], kt == 0, kt == 7, [wb_] + bXbc, bb)
                    P.op("dve", _mk("tensor_tensor", out=g1m[ri][:, i2, :], in0=bk[:, :], in1=g1m[ri][:, i2, :], op=ALU.mult),
                         reads=[bb, bG1], writes=[bG1])
                wv, wb_ = slab_load(wso_v[:, :, pi * 256:(pi + 1) * 256], (16, 256))
                for i2 in range(2):
                    bk, bb = pb()
                    for kt in range(16):
                        mm(bk[:, :], wv[:, kt, i2 * 128:(i2 + 1) * 128], ynT[:, kt, :], kt == 0, kt == 15, [wb_] + bXx, bb)
                    P.op("dve", _mk("tensor_tensor", out=g0t[ri][:, i2, :], in0=bk[:, :], in1=g0t[ri][:, i2, :], op=ALU.mult),
                         reads=[bb, bG0], writes=[bG0])
                    P.op("pool", _mk("tensor_tensor",
                        out=mergedT[:, 2 * pi + i2, :], in0=g0t[ri][:, i2, :], in1=g1m[ri][:, i2, :], op=ALU.add),
                        reads=[bG0, bG1], writes=[bMg])
            if LIMIT == 3:
                break
            wmx = []
            for hf in range(2):
                wmx.append(slab_load(wmix_v[:, :, hf * 512:(hf + 1) * 512], (8, 512)))
            bXt, bH, bHbf, bHT, bSt = bf("xt32"), bf("hh"), bf("hbf"), bf("hT"), bf("st4")
            bLg, bOh, bMm, bMb, bTt, bDf, bEx = bf("lg"), bf("oh"), bf("Mm"), bf("Mb"), bf("Tt"), bf("destf"), bf("exv")
            for ch in range(4):
                c = g * 4 + ch
                cols = slice(ch * 128, (ch + 1) * 128)
                rows = slice(c * 128, (c + 1) * 128)
                P.op("sp", _mk("dma_start", out=xt32[:, :], in_=x_tok[rows, :]), writes=[bXt], dma="xt32")
                for hf in range(2):
                    wv, wb_ = wmx[hf]
                    bk, bb = pb()
                    for kt in range(8):
                        mm(bk[:, :], mergedT[:, kt, cols], wv[:, kt, :], kt == 0, kt == 7, [bMg, wb_], bb)
                    hs = slice(hf * 512, (hf + 1) * 512)
                    P.op("dve", _mk("scalar_tensor_tensor",
                        out=hh[:, hs], in0=xt32[:, hs], scalar=ALPHA, in1=bk[:, :], op0=ALU.mult, op1=ALU.add),
                        reads=[bXt, bb], writes=[bH])
                emit_ln(P, hh, st4, hT, bH, bSt, bHT, lnrep[:, 0, :], lnrep[:, 1, :], bf("lnrep"))
                P.op("sp", _mk("dma_start", out=hbuf[rows, :], in_=hh[:, :]), reads=[bH], writes=[bHB], dma="hst", nowaw=True)
                P.op("act", _mk("activation", func=AF.Copy, out=hbf[:, :], in_=hh[:, :]), reads=[bH], writes=[bHbf])
                for b2 in range(2):
                    bk, bb = pb()
                    for j in range(4):
                        jj = b2 * 4 + j
                        tr(bk[:, j * 128:(j + 1) * 128], hh[:, jj * 128:(jj + 1) * 128], ident_f, [bH] + CB, bb)
                    P.op("act", _mk("activation", func=AF.Copy, out=hT[:, b2 * 4:(b2 + 1) * 4, :], in_=v3(bk[:, :], 128)), reads=[bb], writes=[bHT])
                bk, bb = pb()
                for kt in range(8):
                    mm(bk[:, 0:32], hT[:, kt, :], wrt[:, kt, :], kt == 0, kt == 7, [bHT, bf("wrt")], bb)
                P.op("dve", _mk("tensor_tensor", out=lg[:, :], in0=bk[:, 0:32], in1=brt_rep, op=ALU.add), reads=[bb, bf("rep32")], writes=[bLg])
                for k in range(4):
                    P.op("dve", _mk("reduce_max", out=st4[:, 8 + k:9 + k], in_=lg[:, :], axis=AX.X), reads=[bLg], writes=[bSt])
                    P.op("dve", _mk("tensor_scalar", out=oh[:, k, :], in0=lg[:, :], scalar1=st4[:, 8 + k:9 + k], scalar2=0.0,
                                                               op0=ALU.is_equal, op1=ALU.add), reads=[bLg, bSt], writes=[bOh])
                    P.op("dve", _mk("scalar_tensor_tensor", out=lg[:, :], in0=oh[:, k, :], scalar=-1e9, in1=lg[:, :], op0=ALU.mult, op1=ALU.add),
                         reads=[bOh, bLg], writes=[bLg])
                P.op("dve", _mk("tensor_tensor", out=Mm[:, :], in0=oh[:, 0, :], in1=oh[:, 1, :], op=ALU.add), reads=[bOh], writes=[bMm])
                P.op("dve", _mk("tensor_tensor", out=Mm[:, :], in0=Mm[:, :], in1=oh[:, 2, :], op=ALU.add), reads=[bOh, bMm], writes=[bMm])
                P.op("dve", _mk("tensor_tensor", out=Mb[:, :], in0=Mm[:, :], in1=oh[:, 3, :], op=ALU.add), reads=[bOh, bMm], writes=[bMb])
                P.op("dve", _mk("tensor_scalar", out=st4[:, 13:14], in0=st4[:, 8:9], scalar1=-1.0, scalar2=0.0, op0=ALU.mult, op1=ALU.add),
                     reads=[bSt], writes=[bSt])
                P.op("dve", _mk("memset", st4[:, 12:13], 0.0), reads=[bSt], writes=[bSt])
                P.op("act", _mk("activation", out=exv[:, :], in_=st4[:, 8:12], func=AF.Exp, bias=st4[:, 13:14], accum_out=st4[:, 12:13]),
                     reads=[bSt], writes=[bSt, bEx])
                P.op("dve", _mk("reciprocal", out=st4[:, 14:15], in_=st4[:, 12:13]), reads=[bSt], writes=[bSt])
                P.op("dve", _mk("tensor_scalar", out=gatew[:, c, :], in0=exv[:, :], scalar1=st4[:, 14:15], scalar2=0.0, op0=ALU.mult, op1=ALU.add), reads=[bSt, bEx], writes=[bGw])
                bk, bb = pb()
                mm(bk[:, 0:32], stri_b, Mb[:, :], True, True, [bMb] + CB, bb)
                mm(bk[:, 32:64], ones_b[:, :], Mb[:, :], True, True, [bMb] + CB, bb)
                P.op("dve", _mk("tensor_tensor", out=Tt[:, 0, :], in0=bk[:, 0:32], in1=cnt[:, :], op=ALU.add), reads=[bb, bf("cnt")], writes=[bTt])
                P.op("dve", _mk("tensor_scalar", out=Tt[:, 1, :], in0=Tt[:, 0, :], scalar1=CAP - 0.5, scalar2=1.0, op0=ALU.is_ge, op1=ALU.mult),
                     reads=[bTt], writes=[bTt])
                P.op("dve", _mk("tensor_tensor", out=Tt[:, 0, :], in0=Tt[:, 0, :], in1=ebase, op=ALU.add), reads=[bTt, bf("rep32")], writes=[bTt])
                P.op("dve", _mk("tensor_tensor", out=Tt[:, 2, :], in0=dumpT, in1=Tt[:, 0, :], op=ALU.subtract), reads=[bTt, bf("rep32")], writes=[bTt])
                P.op("dve", _mk("tensor_tensor", out=Tt[:, 2, :], in0=Tt[:, 2, :], in1=Tt[:, 1, :], op=ALU.mult), reads=[bTt], writes=[bTt])
                P.op("dve", _mk("tensor_tensor", out=Tt[:, 0, :], in0=Tt[:, 0, :], in1=Tt[:, 2, :], op=ALU.add), reads=[bTt], writes=[bTt])
                P.op("dve", _mk("tensor_tensor", out=cnt[:, :], in0=cnt[:, :], in1=bk[:, 32:64], op=ALU.add), reads=[bb, bf("cnt")], writes=[bf("cnt")])
                for k in range(4):
                    P.op("dve", _mk("tensor_tensor", out=Tt[:, 2, :], in0=oh[:, k, :], in1=Tt[:, 0, :], op=ALU.mult), reads=[bOh, bTt], writes=[bTt])
                    P.op("dve", _mk("reduce_sum", out=destf[:, k:k + 1], in_=Tt[:, 2, :], axis=AX.X), reads=[bTt], writes=[bDf])
                P.op("dve", _mk("tensor_copy", out=dest[:, c, :], in_=destf[:, :]), reads=[bDf], writes=[bDest])
                for k in range(0 if NO_SCATTER else 4):
                    P.op("pool", _mk("indirect_dma_start",
                        out=Xg[:, :], out_offset=bass.IndirectOffsetOnAxis(ap=dest2[:, c * 4 + k:c * 4 + k + 1], axis=0), in_=hbf[:, :], in_offset=None), reads=[bHbf, bDest], writes=[bXG], dma="sc", nowaw=not first_sc[0])
                    first_sc[0] = False
            P.barrier(extra=[bXt, bH, bHbf])

        aoff[0] = 0
        NE_ = 0 if STOP == 1 else NEXP
        NC3 = 0 if STOP in (1, 2) else NGROUPS * 4
        wgu = [alloc([128, 8, 2048], BF16) for _ in range(2)]
        wdn = [alloc([128, 8, 1024], BF16) for _ in range(2)]
        bdr = [alloc([128, 1024], F32) for _ in range(2)]
        xgt = [alloc([128, 1024], BF16) for _ in range(2)]
        XgT = alloc([128, 8, CAP], BF16)
        actT = alloc([128, 8, CAP], BF16)
        glu = [alloc([128, 384], F32) for _ in range(2)]
        sig = [alloc([128, 384], F32) for _ in range(2)]
        lin = [alloc([128, 384], F32) for _ in range(2)]
        Yt = [alloc([128, 1024], F32) for _ in range(2)]
        p2_end = aoff[0]
        bWgu, bWdn, bBdr = [bf("wgu0"), bf("wgu1")], [bf("wdn0"), bf("wdn1")], [bf("bdr0"), bf("bdr1")]
        bXgt = [bf("xgt0"), bf("xgt1")]
        bXgT, bActT = bf("XgT"), bf("actT")
        bGlu, bSig, bLin = [bf("glu0"), bf("glu1")], [bf("sig0"), bf("sig1")], [bf("lin0"), bf("lin1")]
        bYt = [bf("Yt0"), bf("Yt1")]

        def expert_loads(e_):
            s = e_ % 2
            for q in range(4):
                P.op("pool", _mk("dma_start",
                    out=wgu[s][:, :, q * 512:(q + 1) * 512], in_=w_gu[e_].rearrange("(kt p) c -> p kt c", p=128)[:, :, q * 512:(q + 1) * 512]),
                    writes=[bWgu[s]], dma="wgu%d" % s, nowaw=(q > 0))
            for q in range(2):
                P.op("pool", _mk("dma_start",
                    out=wdn[s][:, :, q * 512:(q + 1) * 512], in_=w_dn[e_].rearrange("(kt p) c -> p kt c", p=128)[:, :, q * 512:(q + 1) * 512]),
                    writes=[bWdn[s]], dma="wdn%d" % s, nowaw=(q > 0))
            P.op("sp", _mk("dma_start", out=bdr[s][:, :], in_=bd_rep[e_]), writes=[bBdr[s]], dma="bdr%d" % s)

        if NE_:
            expert_loads(0)
        for e_ in range(NE_):
            s = e_ % 2
            if e_ + 1 < NE_:
                expert_loads(e_ + 1)
            for stl in range(6):
                xs_ = stl % 2
                r0 = e_ * CAP + stl * 128
                P.op("sp", _mk("dma_start", out=xgt[xs_][:, :], in_=Xg[r0:r0 + 128, :]), reads=[bXG], writes=[bXgt[xs_]], dma="xgt%d" % xs_)
                bk, bb = pb()
                pbf = bk[:, :].bitcast(BF16)
                for j in range(8):
                    tr(pbf[:, j * 128:(j + 1) * 128], xgt[xs_][:, j * 128:(j + 1) * 128], ident_b, [bXgt[xs_]] + CB, bb)
                P.op("act", _mk("activation", func=AF.Copy, out=XgT[:, :, stl * 128:(stl + 1) * 128], in_=v3(pbf, 128)), reads=[bb], writes=[bXgT])
            for fi in range(8):
                for hf in range(2):
                    cs = slice(hf * 384, (hf + 1) * 384)
                    r = (fi * 2 + hf) % 2
                    bkG, bbG = pb()
                    for kt in range(8):
                        mm(bkG[:, 0:384], wgu[s][:, kt, fi * 128:(fi + 1) * 128], XgT[:, kt, cs], kt == 0, kt == 7, [bWgu[s], bXgT], bbG)
                    bkL, bbL = pb()
                    for kt in range(8):
                        mm(bkL[:, 0:384], wgu[s][:, kt, 1024 + fi * 128:1024 + (fi + 1) * 128], XgT[:, kt, cs], kt == 0, kt == 7, [bWgu[s], bXgT], bbL)
                    P.op("dve", _mk("tensor_scalar",
                        out=glu[r][:, :], in0=bkG[:, 0:384], scalar1=bgu[:, e_, fi:fi + 1], scalar2=7.0, op0=ALU.add, op1=ALU.min),
                        reads=[bbG, bf("bgu")], writes=[bGlu[r]])
                    P.op("act", _mk("activation", out=sig[r][:, :], in_=glu[r][:, :], func=AF.Sigmoid, scale=1.702), reads=[bGlu[r]], writes=[bSig[r]])
                    P.op("dve", _mk("tensor_scalar",
                        out=lin[r][:, :], in0=bkL[:, 0:384], scalar1=bgu[:, e_, 8 + fi:9 + fi], scalar2=7.0, op0=ALU.add, op1=ALU.min),
                        reads=[bbL, bf("bgu")], writes=[bLin[r]])
                    P.op("dve", _mk("tensor_scalar", out=lin[r][:, :], in0=lin[r][:, :], scalar1=-7.0, scalar2=1.0, op0=ALU.max, op1=ALU.add),
                         reads=[bLin[r]], writes=[bLin[r]])
                    P.op("pool", _mk("tensor_tensor", out=sig[r][:, :], in0=sig[r][:, :], in1=glu[r][:, :], op=ALU.mult),
                         reads=[bSig[r], bGlu[r]], writes=[bSig[r]])
                    P.op("pool", _mk("tensor_tensor", out=actT[:, fi, cs], in0=sig[r][:, :], in1=lin[r][:, :], op=ALU.mult),
                         reads=[bSig[r], bLin[r]], writes=[bActT])
            for stl in range(6):
                ys = stl % 2
                r0 = e_ * CAP + stl * 128
                for hf in range(2):
                    hs = slice(hf * 512, (hf + 1) * 512)
                    bk, bb = pb()
                    for ft in range(8):
                        mm(bk[:, :], actT[:, ft, stl * 128:(stl + 1) * 128], wdn[s][:, ft, hs], ft == 0, ft == 7, [bActT, bWdn[s]], bb)
                    P.op("dve", _mk("tensor_tensor", out=Yt[ys][:, hs], in0=bk[:, :], in1=bdr[s][:, hs], op=ALU.add),
                         reads=[bb, bBdr[s]], writes=[bYt[ys]])
                P.op("sp", _mk("dma_start", out=Yg[r0:r0 + 128, :], in_=Yt[ys][:, :]), reads=[bYt[ys]], writes=[bYG], dma="yst%d" % ys, nowaw=True)
        P.barrier(extra=bYt + bXgt)

        aoff[0] = 0
        yk = [[alloc([128, 1024], F32) for _ in range(4)] for _ in range(2)]
        h3 = [alloc([128, 1024], F32) for _ in range(2)]
        ac3 = [alloc([128, 1024], F32) for _ in range(2)]
        jk3 = alloc([128, 1024], F32)
        st3 = alloc([128, 16], F32)
        bYk = [[bf("yk%d_%d" % (r, k)) for k in range(4)] for r in range(2)]
        bH3, bAc3 = [bf("h3_0"), bf("h3_1")], [bf("ac3_0"), bf("ac3_1")]
        bJk3, bSt3 = bf("jk3"), bf("st3")
        for c in range(NC3):
            r = c % 2
            rows = slice(c * 128, (c + 1) * 128)
            for k in range(4):
                P.op("pool", _mk("indirect_dma_start",
                    out=yk[r][k][:, :], out_offset=None, in_=Yg[:, :], in_offset=bass.IndirectOffsetOnAxis(ap=dest2[:, c * 4 + k:c * 4 + k + 1], axis=0)), reads=[bYG, bDest], writes=[bYk[r][k]], dma="yk%d_%d" % (r, k))
            P.op("sp", _mk("dma_start", out=h3[r][:, :], in_=hbuf[rows, :]), reads=[bHB], writes=[bH3[r]], dma="h3_%d" % r)
            a3 = ac3[r]
            P.op("dve", _mk("tensor_scalar", out=a3[:, :], in0=yk[r][0][:, :], scalar1=gatew[:, c, 0:1], scalar2=0.0, op0=ALU.mult, op1=ALU.add),
                 reads=[bYk[r][0], bGw], writes=[bAc3[r]])
            for k in range(1, 4):
                P.op("dve", _mk("scalar_tensor_tensor",
                    out=a3[:, :], in0=yk[r][k][:, :], scalar=gatew[:, c, k:k + 1], in1=a3[:, :], op0=ALU.mult, op1=ALU.add),
                    reads=[bYk[r][k], bGw, bAc3[r]], writes=[bAc3[r]])
            P.op("dve", _mk("scalar_tensor_tensor", out=a3[:, :], in0=h3[r][:, :], scalar=ALPHA, in1=a3[:, :], op0=ALU.mult, op1=ALU.add),
                 reads=[bH3[r], bAc3[r]], writes=[bAc3[r]])
            emit_ln(P, a3, st3, jk3, bAc3[r], bSt3, bJk3, lnrep[:, 2, :], lnrep[:, 3, :], bf("lnrep"))
            P.op("sp", _mk("dma_start", out=out_d[rows, :], in_=a3[:, :]), reads=[bAc3[r]], writes=[bOUT], dma="out%d" % r, nowaw=True)
        P.op("sp", _mk("nop", ), reads=[bOUT, bHB, bXT] + sl_b, force=True)
        P.run()
    return nc


def emit_ln(P, x, st, junk, bX, bSt, bJ, g_rep, b_rep, bLn):
    jv = junk if len(junk.shape) == 2 else junk.rearrange("p a b -> p (a b)")
    P.op("dve", _mk("memset", st[:, 0:4], 0.0), reads=[bSt], writes=[bSt])
    P.op("act", _mk("activation", out=jv[:, 0:1024], in_=x[:, :], func=AF.Identity, accum_out=st[:, 0:1]), reads=[bX, bSt], writes=[bSt, bJ])
    P.op("dve", _mk("tensor_scalar", out=st[:, 1:2], in0=st[:, 0:1], scalar1=-1.0 / 1024.0, scalar2=0.0, op0=ALU.mult, op1=ALU.add),
         reads=[bSt], writes=[bSt])
    P.op("act", _mk("activation", out=jv[:, 0:1024], in_=x[:, :], func=AF.Square, bias=st[:, 1:2], accum_out=st[:, 2:3]),
         reads=[bX, bSt], writes=[bSt, bJ])
    P.op("act", _mk("activation", out=st[:, 3:4], in_=st[:, 2:3], func=AF.Ln, scale=1.0 / 1024.0, bias=1e-5), reads=[bSt], writes=[bSt])
    P.op("act", _mk("activation", out=st[:, 4:5], in_=st[:, 3:4], func=AF.Exp, scale=-0.5), reads=[bSt], writes=[bSt])
    P.op("dve", _mk("tensor_scalar", out=x[:, :], in0=x[:, :], scalar1=st[:, 1:2], scalar2=st[:, 4:5], op0=ALU.add, op1=ALU.mult),
         reads=[bX, bSt], writes=[bX])
    P.op("pool", _mk("tensor_tensor", out=x[:, :], in0=x[:, :], in1=g_rep, op=ALU.mult), reads=[bX, bLn], writes=[bX])
    P.op("pool", _mk("tensor_tensor", out=x[:, :], in0=x[:, :], in1=b_rep, op=ALU.add), reads=[bX, bLn], writes=[bX])


def _consts():
    k = np.arange(128)[:, None]
    t = np.arange(128)[None, :]
    ident = (k == t).astype(np.float32)
    tri = (k <= t).astype(np.float32)
    stri = (k < t).astype(np.float32)
    negm_cur = np.where(k <= t, 0.0, NEG).astype(np.float32)
    negm_prev = np.where(k > t, 0.0, NEG).astype(np.float32)
    return np.concatenate([ident, tri, stri, negm_cur, negm_prev], axis=1)


def _prep(inp):
    f = lambda a: np.ascontiguousarray(np.asarray(a, dtype=np.float32))
    w_in = f(inp["w_in"])[0]
    o = np.cumsum((0, 2048, 3072, 32, 1024, 256, 256, 2048))
    z, xbc, dt, q, k, v, gts = [w_in[:, o[i]:o[i + 1]] for i in range(7)]
    kdup = np.concatenate([np.concatenate([k[:, g * 64:(g + 1) * 64]] * 2, axis=1) for g in range(4)], axis=1)
    w_fm = np.concatenate([xbc, q, kdup], axis=1)
    w_vdt = np.concatenate([v, dt], axis=1)
    g0, g1 = gts[:, :1024], gts[:, 1024:]
    w_gp = np.concatenate([np.concatenate([g0[:, p * 256:(p + 1) * 256], g1[:, p * 256:(p + 1) * 256]], axis=1) for p in range(4)], axis=1)
    pl = lambda vec, nt: f(vec).reshape(nt, 128).T
    conv_w = f(inp["conv_w"])[0]
    convw_l = np.stack([pl(conv_w[j], 24) for j in range(4)], axis=2).reshape(128, 96)
    rep = lambda vec: np.broadcast_to(f(vec).reshape(1, -1), (128, f(vec).size))
    sinks = np.zeros(32, np.float32)
    sinks[:16] = f(inp["attn_sinks"])[0]
    ebase = (np.arange(32) * CAP).astype(np.float32)
    rep32 = np.concatenate([rep(inp["dt_bias"][0]), rep(inp["a_log"][0]), rep(inp["d_skip"][0]), rep(inp["b_router"][0]), rep(sinks), rep(ebase), np.broadcast_to((NEXP * CAP + np.arange(128, dtype=np.float32))[:, None], (128, 32))], axis=1)
    ln_rep = np.concatenate([rep(inp["ln1_g"][0]), rep(inp["ln1_b"][0]), rep(inp["ln2_g"][0]), rep(inp["ln2_b"][0])], axis=1)
    bgu = f(inp["b_gate_up"])[0]
    bgu_l = np.stack([pl(bgu[e], 16) for e in range(NEXP)], axis=1).reshape(128, NEXP * 16)
    bd = f(inp["b_down"])[0]
    shared = dict(
        w_fm=f(w_fm), w_z=f(z), w_vdt=f(w_vdt), w_gp=f(w_gp), w_so=f(inp["w_ssm_out"])[0], w_ao=f(inp["w_attn_out"])[0],
        w_mix=f(inp["w_mix_out"])[0], w_rt=f(inp["w_router"])[0], w_gu=f(inp["w_gate_up"])[0], w_dn=f(inp["w_down"])[0],
        bd_rep=f(np.broadcast_to(bd[:, None, :], (NEXP, 128, 1024))),
        convw_l=f(convw_l), convb_l=f(pl(inp["conv_b"][0], 24)), bgate_l=f(pl(inp["b_gates"][0], 16)),
        normw_l=f(pl(inp["ssm_norm_w"][0], 16)), bgu_l=f(bgu_l), rep32=f(rep32), ln_rep=f(ln_rep), cst=f(_consts()))
    x = f(inp["x"])
    maps = []
    for c in range(NCORES):
        xt = x[2 * c:2 * c + 2].reshape(NT, 1024)
        m = dict(shared)
        m["x_tok"] = np.ascontiguousarray(xt)
        m["xT"] = np.ascontiguousarray(xt.T)
        maps.append(m)
    return maps


def kernel(**inputs):
    maps = _prep(inputs)
    nc = build_nc()
    res = run_bass_kernel_spmd(nc, maps, core_ids=list(range(NCORES)))
    out = np.concatenate([np.asarray(r["out"]).reshape(2, 2048, 1024) for r in res.results], axis=0)
    return out.astype(np.float32)
```

```python
import contextlib
import numpy as np
import concourse.bass as bass
import concourse.mybir as mybir
from concourse.bass_utils import run_bass_kernel_spmd

F32 = mybir.dt.float32
BF16 = mybir.dt.bfloat16
I32 = mybir.dt.int32
AF = mybir.ActivationFunctionType
ALU = mybir.AluOpType
AX = mybir.AxisListType

NCORES = 8
NT = 4096
NCH = 32
NG = 8
CAP = 768
NEXP = 32
ALPHA = 2.0 ** 0.25
NEG = -30000.0
DEBUG_H = False
STOP = 0
NO_SCATTER = False
NGROUPS = NG
LIMIT = 0
DBGV = {}
MAXOPS = 0
NOBAR = False
LIMC = 4


class Buf:
    __slots__ = ("name", "w", "r", "excl")

    def __init__(self, name, excl=False):
        self.name = name
        self.w = None
        self.r = []
        self.excl = excl


class Prog:
    def __init__(self, nc, stack):
        self.nc = nc
        self.stack = stack
        self.ops = []
        self.sems = {}
        self.last = {}
        self.barbuf = {e: Buf("bar_" + e) for e in ("pe", "act", "dve", "pool")}
        self.barx = Buf("barx")
        self.pending = {}

    def op(self, eng, fn, reads=(), writes=(), dma=None, nowaw=False, force=False, final=False):
        if MAXOPS and len(self.ops) >= MAXOPS and not final:
            return len(self.ops) - 1
        i = len(self.ops)
        deps = set()
        for b in reads:
            if b.w is not None:
                deps.add(b.w)
            if b.excl:
                deps.update(r for r in b.r if self.ops[r]["eng"] != eng)
        for b in writes:
            if b.w is not None and not nowaw:
                deps.add(b.w)
            deps.update(b.r)
        for b in reads:
            b.r.append(i)
        for b in writes:
            b.w = i
            if not nowaw:
                b.r = []
        if self.pending.get(eng):
            deps.update(self.pending.pop(eng))
            force = True
        deps.discard(i)
        self.ops.append(dict(eng=eng, fn=fn, deps=deps, dma=dma, needed=False, token=None, waits=[], force=force))
        if dma is None:
            self.last[eng] = i
        return i

    def barrier(self, extra=()):
        if NOBAR:
            return
        ids = set(self.last.values())
        for b in extra:
            if b.w is not None:
                ids.add(b.w)
            ids.update(b.r)
        for e in ("pe", "act", "dve", "pool", "sp"):
            self.pending.setdefault(e, set()).update(ids)

    def _skip(self, op, D):
        return (D["dma"] is None and op["dma"] is None and D["eng"] == "pe" and op["eng"] == "pe"
                and not op["force"] and not D["force"])

    def finish(self):
        lastd = {}
        for i, op in enumerate(self.ops):
            if op["dma"] is not None:
                lastd[op["dma"]] = i
        lastv = list(self.last.values())
        i = self.op("sp", lambda en: en.nop(), force=True, final=True)
        self.ops[i]["deps"].update(lastd.values())
        self.ops[i]["deps"].update(lastv)
        self.ops[i]["deps"].discard(i)

    def run(self):
        self.finish()
        ops = self.ops
        for op in ops:
            for d in op["deps"]:
                if not self._skip(op, ops[d]):
                    ops[d]["needed"] = True
        cnt = {}
        for op in ops:
            if op["dma"] is not None:
                k = ("d", op["dma"])
                cnt[k] = cnt.get(k, 0) + 16
                op["token"] = (k, cnt[k])
            elif op["needed"]:
                k = ("e", op["eng"])
                cnt[k] = cnt.get(k, 0) + 1
                op["token"] = (k, cnt[k])
        waited = {}
        for op in ops:
            ws = {}
            for d in op["deps"]:
                D = ops[d]
                if self._skip(op, D):
                    continue
                k, v = D["token"]
                ws[k] = max(ws.get(k, 0), v)
            wd = waited.setdefault(op["eng"], {})
            op["waits"] = [(k, v) for k, v in ws.items() if wd.get(k, 0) < v]
            for k, v in op["waits"]:
                wd[k] = v
        for op in ops:
            if op["token"] and op["token"][0] not in self.sems:
                k = op["token"][0]
                self.sems[k] = self.stack.enter_context(self.nc.semaphore("s_%s_%s" % k))
        engs = {"pe": "tensor", "act": "scalar", "dve": "vector", "pool": "gpsimd", "sp": "sync"}
        with self.nc.Block() as block:
            for en, attr in engs.items():
                mine = [op for op in ops if op["eng"] == en]
                if not mine:
                    continue

                def body(e, mine=mine):
                    for op in mine:
                        for k, v in op["waits"]:
                            e.wait_ge(self.sems[k], v)
                        try:
                            ins = op["fn"](e)
                        except Exception:
                            print("FAILED OP eng", op["eng"], "dma", op["dma"], "nsems", len(self.sems), "free", self.nc.free_len())
                            raise
                        if op["token"]:
                            ins.then_inc(self.sems[op["token"][0]], 16 if op["dma"] is not None else 1)

                getattr(block, attr)(body)


def _mk(meth, *a, **k):
    return lambda e: getattr(e, meth)(*a, **k)


def v3(ap, b):
    return ap.rearrange("p (a b) -> p a b", b=b)


def bc(ap, m):
    p, n = ap.shape
    return ap.unsqueeze(2).to_broadcast([p, n, m])


def build_nc():
    nc = bass.Bass("TRN2", target_bir_lowering=False)
    din = lambda name, shape, dt=F32: nc.dram_tensor(name, list(shape), dt, kind="ExternalInput").ap()
    x_tok = din("x_tok", [NT, 1024])
    xT_d = din("xT", [1024, NT])
    w_fm = din("w_fm", [1024, 4608])
    w_z = din("w_z", [1024, 2048])
    w_vdt = din("w_vdt", [1024, 288])
    w_gp = din("w_gp", [1024, 2048])
    w_so = din("w_so", [2048, 1024])
    w_ao = din("w_ao", [1024, 1024])
    w_mix = din("w_mix", [1024, 1024])
    w_rt = din("w_rt", [1024, 32])
    NXD = 1 if STOP == 1 else NEXP
    w_gu = din("w_gu", [NXD, 1024, 2048])
    w_dn = din("w_dn", [NXD, 1024, 1024])
    bd_rep = din("bd_rep", [NXD, 128, 1024])
    convw_d = din("convw_l", [128, 24 * 4])
    convb_d = din("convb_l", [128, 24])
    bgate_d = din("bgate_l", [128, 16])
    normw_d = din("normw_l", [128, 16])
    bgu_d = din("bgu_l", [128, NEXP * 16])
    rep32_d = din("rep32", [128, 7 * 32])
    lnrep_d = din("ln_rep", [128, 4 * 1024])
    cst_d = din("cst", [128, 5 * 128])
    out_d = nc.dram_tensor("out", [NT, 1024], F32, kind="ExternalOutput").ap()
    hkind = "ExternalOutput" if DEBUG_H else "Internal"
    hbuf = nc.dram_tensor("hbuf", [NT, 1024], F32, kind=hkind).ap()
    Xg = nc.dram_tensor("Xg", [NEXP * CAP + 128, 1024], BF16, kind="Internal").ap()
    Yg = nc.dram_tensor("Yg", [NEXP * CAP + 128, 1024], F32, kind="Internal").ap()

    with contextlib.ExitStack() as st:
        P = Prog(nc, st)
        sbt = lambda name, shape, dt: st.enter_context(nc.sbuf_tensor("sb_" + name, list(shape), dt))
        banks = [st.enter_context(nc.psum_tensor("ps%d" % i, [128, 512], F32)) for i in range(8)]
        bbufs = [Buf("ps%d" % i, excl=True) for i in range(8)]
        bstate = [0]

        def pb():
            i = bstate[0] % 8
            bstate[0] += 1
            return banks[i], bbufs[i]

        cst = sbt("cst", [128, 640], F32)
        ident_f = cst[:, 0:128]
        tri = cst[:, 128:256]
        cstb = sbt("cstb", [128, 640], BF16)
        ident_b = cstb[:, 0:128]
        stri_b = cstb[:, 256:384]
        ones_f = sbt("ones_f", [128, 128], F32)
        ones_b = sbt("ones_b", [128, 128], BF16)
        negm = sbt("negm", [128, 2, 512], BF16)
        convw = sbt("convw", [128, 24, 4], F32)
        convb = sbt("convb", [128, 24], F32)
        bgate = sbt("bgate", [128, 16], F32)
        normw = sbt("normw", [128, 16], F32)
        bgu = sbt("bgu", [128, NEXP, 16], F32)
        rep32 = sbt("rep32", [128, 7, 32], F32)
        lnrep = sbt("lnrep", [128, 4, 1024], F32)
        A_rep = sbt("A_rep", [128, 32], F32)
        esink = sbt("esink", [128, 16], F32)
        cnt = sbt("cnt", [128, 32], F32)
        dest2 = sbt("dest", [128, NCH * 4], I32)
        dest = v3(dest2[:, :], 4)
        gatew = sbt("gatew", [128, NCH, 4], F32)
        wrt = sbt("wrt", [128, 8, 32], F32)
        wvdt = sbt("wvdt", [128, 8, 288], BF16)
        B = {}

        def bf(n):
            if n not in B:
                B[n] = Buf(n)
            return B[n]

        ld = lambda dst, src, name: P.op("sp", _mk("dma_start", out=dst, in_=src), writes=[bf(name)], dma="c_" + name)
        ld(cst[:, :], cst_d, "cst")
        ld(convw[:, :, :], v3(convw_d, 4), "convw")
        ld(convb[:, :], convb_d, "convb")
        ld(bgate[:, :], bgate_d, "bgate")
        ld(normw[:, :], normw_d, "normw")
        ld(bgu[:, :, :], v3(bgu_d, 16), "bgu")
        ld(rep32[:, :, :], v3(rep32_d, 32), "rep32")
        ld(lnrep[:, :, :], v3(lnrep_d, 1024), "lnrep")
        ld(wrt[:, :, :], w_rt.rearrange("(kt p) c -> p kt c", p=128), "wrt")
        P.op("pool", _mk("dma_start", out=wvdt[:, :, :], in_=w_vdt.rearrange("(kt p) c -> p kt c", p=128)),
             writes=[bf("wvdt")], dma="c_wvdt")
        P.op("dve", _mk("tensor_copy", out=cstb[:, :], in_=cst[:, :]), reads=[bf("cst")], writes=[bf("cstb")])
        P.op("dve", _mk("memset", ones_f[:, :], 1.0), writes=[bf("ones_f")])
        P.op("dve", _mk("memset", ones_b[:, :], 1.0), writes=[bf("ones_b")])
        P.op("dve", _mk("memset", cnt[:, :], 0.0), writes=[bf("cnt")])
        for kt_ in range(2):
            for r_ in range(4):
                c0_ = 512 if kt_ == 0 else 384
                P.op("dve", _mk("tensor_copy",
                    out=negm[:, kt_, r_ * 128:(r_ + 1) * 128], in_=cst[:, c0_:c0_ + 128]),
                    reads=[bf("cst")], writes=[bf("negm")])
        P.op("act", _mk("activation", out=A_rep[:, :], in_=rep32[:, 1, :], func=AF.Exp), reads=[bf("rep32")], writes=[bf("A_rep")])
        P.op("dve", _mk("tensor_scalar", out=A_rep[:, :], in0=A_rep[:, :], scalar1=-1.0, scalar2=0.0, op0=ALU.mult, op1=ALU.add),
             reads=[bf("A_rep")], writes=[bf("A_rep")])
        P.op("act", _mk("activation", out=esink[:, :], in_=rep32[:, 4, 0:16], func=AF.Exp), reads=[bf("rep32")], writes=[bf("esink")])
        dtb_rep = rep32[:, 0, :]
        D_rep = rep32[:, 2, :]
        brt_rep = rep32[:, 3, :]
        ebase = rep32[:, 5, :]
        dumpT = rep32[:, 6, :]
        CB = [bf("cst"), bf("cstb"), bf("negm"), bf("ones_f"), bf("ones_b")]

        ARW = 43200
        arena = sbt("arena", [128, ARW], F32)
        aoff = [0]

        def alloc(shape, dt, name=None):
            if name:
                DBGV[name] = (aoff[0], tuple(shape), dt)
            n = int(np.prod(shape[1:]))
            words = n if dt in (F32, I32) else (n + 1) // 2
            words = (words + 7) // 8 * 8
            v = arena[0:shape[0], aoff[0]:aoff[0] + words]
            aoff[0] += words
            assert aoff[0] <= ARW, aoff[0]
            if dt != F32:
                v = v.bitcast(dt)
            v = v[:, 0:n]
            if len(shape) == 3:
                v = v3(v, shape[2])
            elif len(shape) == 4:
                v = v.rearrange("p (a b c) -> p a b c", b=shape[2], c=shape[3])
            return v

        xTb = alloc([128, 8, 512], BF16, "xTb")
        slabs = [alloc([128, 4096], BF16) for _ in range(3)]
        sl_b = [Buf("slab%d" % i) for i in range(3)]
        slst = [0]
        xbc = alloc([128, 24, 512], BF16, "xbc")
        QT = alloc([128, 2, 8, 512], BF16, "QT")
        KT = alloc([128, 4, 640], BF16, "KT")
        Vaug = alloc([128, 2, 4, 68], BF16, "Vaug")
        sz = alloc([128, 4, 2048], BF16, "sz")
        hist = alloc([128, 24, 3], F32)
        S = alloc([128, 2048], F32, "S")
        Sbf = alloc([128, 2048], BF16)
        region0 = aoff[0]
        U = [alloc([128, 515], F32) for _ in range(2)]
        acc = [alloc([128, 512], F32) for _ in range(2)]
        aoff[0] = region0
        sm = alloc([128, 12, 32], F32, "sm")
        xtok = alloc([128, 2048], BF16, "xtok")
        xs = alloc([128, 2048], BF16, "xs")
        xsw = alloc([128, 2048], BF16)
        Btok = alloc([128, 4, 128], BF16, "Btok")
        cbT = alloc([128, 4, 128], F32, "cbT")
        LT = [alloc([128, 512], F32) for _ in range(2)]
        MT = [alloc([128, 512], BF16) for _ in range(4)]
        t1 = [alloc([128, 512], F32) for _ in range(2)]
        t2 = [alloc([128, 512], F32) for _ in range(2)]
        yz = alloc([128, 2048], F32, "yz")
        junk = alloc([128, 1024], F32)
        yn = alloc([128, 2048], BF16, "yn")
        ss = alloc([128, 8], F32)
        PT = [alloc([128, 2, 512], BF16) for _ in range(2)]
        atok = alloc([128, 1024], BF16, "atok")
        rec = alloc([128, 8], F32)
        regionC_end = aoff[0]
        aoff[0] = region0
        mergedT = alloc([128, 8, 512], BF16, "mergedT")
        g1m = [alloc([128, 2, 512], F32) for _ in range(2)]
        g0t = [alloc([128, 2, 512], F32) for _ in range(2)]
        xt32 = alloc([128, 1024], F32)
        hh = alloc([128, 1024], F32, "hh")
        hbf = alloc([128, 1024], BF16)
        hT = alloc([128, 8, 128], F32)
        st4 = alloc([128, 16], F32)
        lg = alloc([128, 32], F32)
        oh = alloc([128, 4, 32], F32)
        Mm = alloc([128, 32], F32)
        Mb = alloc([128, 32], BF16)
        Tt = alloc([128, 3, 32], F32)
        destf = alloc([128, 4], F32)
        exv = alloc([128, 4], F32)
        regionDE_end = aoff[0]
        p1_end = max(regionC_end, regionDE_end)
        ynT = xbc[:, 0:16, :]
        aT = xbc[:, 16:24, :]

        plan = []
        issued = [0]

        NSL = 27
        wscr = nc.dram_tensor("wscr", [NSL, 128, 4096], BF16, kind="Internal").ap()
        bScr = [Buf("wscr%d" % k_) for k_ in range(NSL)]

        def slab_issue_upto(n):
            while issued[0] < min(n, len(plan)):
                i = issued[0]
                src_ap, (a, b) = plan[i]
                k_ = i % NSL
                sl2 = slabs[i % 3][:, 0:a * b]
                if i < NSL:
                    view = v3(sl2, b)
                    P.op("pool", _mk("dma_start", out=view, in_=src_ap), writes=[sl_b[i % 3]], dma="slab%d" % (i % 3))
                    P.op("sp", _mk("dma_start", out=wscr[k_][:, 0:a * b], in_=sl2), reads=[sl_b[i % 3]], writes=[bScr[k_]],
                         dma="wst%d" % (i % 3))
                else:
                    P.op("sp", _mk("dma_start", out=sl2, in_=wscr[k_][:, 0:a * b]), reads=[bScr[k_]], writes=[sl_b[i % 3]],
                         dma="slabh%d" % (i % 3))
                issued[0] += 1

        def slab_load(src_ap, shape3):
            i = slst[0]
            slst[0] += 1
            assert plan[i][1] == shape3, (i, plan[i][1], shape3)
            slab_issue_upto(i + 2)
            a, b = shape3
            return v3(slabs[i % 3][:, 0:a * b], b), sl_b[i % 3]

        def mm(out, lhsT, rhs, start, stop, reads, wbuf):
            P.op("pe", _mk("matmul", out, lhsT=lhsT, rhs=rhs, start=start, stop=stop), reads=reads, writes=[wbuf])

        def tr(out, in_, ident, reads, wbuf):
            P.op("pe", _mk("transpose", out=out, in_=in_, identity=ident), reads=reads, writes=[wbuf])

        wfm_v = w_fm.rearrange("(kt p) c -> p kt c", p=128)
        wz_v = w_z.rearrange("(kt p) c -> p kt c", p=128)
        wgp_v = w_gp.rearrange("(kt p) c -> p kt c", p=128)
        wso_v = w_so.rearrange("(kt p) c -> p kt c", p=128)
        wao_v = w_ao.rearrange("(kt p) c -> p kt c", p=128)
        wmix_v = w_mix.rearrange("(kt p) c -> p kt c", p=128)
        xT_v = xT_d.rearrange("(kt p) t -> p kt t", p=128)
        for g_ in range(NGROUPS):
            for sl in range(9):
                plan.append((wfm_v[:, :, sl * 512:(sl + 1) * 512], (8, 512)))
            for zs in range(4):
                plan.append((wz_v[:, :, zs * 512:(zs + 1) * 512], (8, 512)))
            for pi in range(4):
                plan.append((wgp_v[:, :, pi * 512:(pi + 1) * 512], (8, 512)))
                plan.append((wao_v[:, :, pi * 256:(pi + 1) * 256], (8, 256)))
                plan.append((wso_v[:, :, pi * 256:(pi + 1) * 256], (16, 256)))
            for hf in range(2):
                plan.append((wmix_v[:, :, hf * 512:(hf + 1) * 512], (8, 512)))

        bXT = bf("xT")
        bXx = [bf("xbc_x%d" % i) for i in range(4)]
        bXbc = [bf("xbc_bc%d" % i) for i in range(4)]
        bQT, bKT, bV = bf("QT"), bf("KT"), [bf("V0"), bf("V1")]
        bSZ = [bf("sz%d" % i) for i in range(4)]
        bHist = [bf("hist%d" % i) for i in range(24)]
        bS = [bf("S%d" % i) for i in range(4)]
        bSb = [bf("Sb%d" % i) for i in range(4)]
        bU = [bf("U0"), bf("U1")]
        bAcc = [bf("acc0"), bf("acc1")]
        PRM = [bf("convw"), bf("convb")]
        bOUT = bf("OUT")
        bHB = bf("hbufd")
        bXG = bf("Xgd")
        bYG = bf("Ygd")
        bDest = bf("dest")
        bGw = bf("gatew")
        P.op("dve", _mk("memset", Vaug[:, :, :, :], 1.0), writes=bV)
        P.op("dve", _mk("memset", QT[:, :, :, :], 0.0), writes=[bQT])
        ztile = sbt("ztile", [128, 1024], BF16)
        P.op("dve", _mk("memset", ztile[:, :], 0.0), writes=[bf("ztile")])
        for r0_ in range(0, NEXP * CAP + 128, 128):
            P.op("sp", _mk("dma_start", out=Xg[r0_:r0_ + 128, :], in_=ztile[:, :]), reads=[bf("ztile")], writes=[bXG], dma="xgz", nowaw=True)
        first_sc = [True]

        for g in range(NGROUPS):
            gi = g % 4
            first_grp = gi == 0
            tok0 = g * 512
            P.op("pool", _mk("dma_start", out=xTb[:, :, :], in_=xT_v[:, :, tok0:tok0 + 512]),
                 writes=[bXT], dma="xT")
            if first_grp:
                P.op("dve", _mk("memset", S[:, :], 0.0), writes=bS)
                P.op("dve", _mk("memset", Sbf[:, :], 0.0), writes=bSb)
            else:
                P.op("pool", _mk("tensor_copy", out=KT[:, :, 0:128], in_=KT[:, :, 512:640]), reads=[bKT], writes=[bKT])
            for sl in range(9):
                wv, wb_ = slab_load(wfm_v[:, :, sl * 512:(sl + 1) * 512], (8, 512))
                for t4 in range(4):
                    ti = sl * 4 + t4
                    bk, bb = pb()
                    for kt in range(8):
                        mm(bk[:, :], wv[:, kt, t4 * 128:(t4 + 1) * 128], xTb[:, kt, :], kt == 0, kt == 7, [wb_, bXT], bb)
                    if ti < 24:
                        u = ti % 2
                        Uu, au = U[u], acc[u]
                        P.op("dve", _mk("tensor_copy", out=Uu[:, 3:515], in_=bk[:, :]), reads=[bb], writes=[bU[u]])
                        if first_grp:
                            P.op("pool", _mk("memset", Uu[:, 0:3], 0.0), writes=[bU[u]])
                        else:
                            P.op("pool", _mk("tensor_copy", out=Uu[:, 0:3], in_=hist[:, ti, :]),
                                 reads=[bHist[ti]], writes=[bU[u]])
                        P.op("dve", _mk("tensor_scalar",
                            out=au[:, :], in0=bk[:, :], scalar1=convw[:, ti, 3:4], scalar2=convb[:, ti:ti + 1], op0=ALU.mult, op1=ALU.add),
                            reads=[bb] + PRM, writes=[bAcc[u]])
                        for j in range(3):
                            P.op("dve", _mk("scalar_tensor_tensor",
                                out=au[:, :], in0=Uu[:, j:j + 512], scalar=convw[:, ti, j:j + 1], in1=au[:, :],
                                op0=ALU.mult, op1=ALU.add), reads=[bU[u], bAcc[u]] + PRM, writes=[bAcc[u]])
                        wl = bXx if ti < 16 else bXbc
                        P.op("act", _mk("activation", out=xbc[:, ti, :], in_=au[:, :], func=AF.Silu),
                             reads=[bAcc[u]], writes=wl)
                        P.op("pool", _mk("tensor_copy", out=hist[:, ti, :], in_=Uu[:, 512:515]),
                             reads=[bU[u]], writes=[bHist[ti]])
                    elif ti < 32:
                        P.op("act", _mk("activation", func=AF.Copy, out=QT[0:64, 0, ti - 24, :], in_=bk[0:64, :]), reads=[bb], writes=[bQT])
                        P.op("act", _mk("activation", func=AF.Copy, out=QT[64:128, 1, ti - 24, :], in_=bk[64:128, :]), reads=[bb], writes=[bQT])
                    else:
                        P.op("dve", _mk("tensor_copy", out=KT[:, ti - 32, 128:640], in_=bk[:, :]), reads=[bb], writes=[bKT])
            for zs in range(4):
                wv, wb_ = slab_load(wz_v[:, :, zs * 512:(zs + 1) * 512], (8, 512))
                for ch in range(4):
                    bk, bb = pb()
                    for kt in range(8):
                        mm(bk[:, :], xTb[:, kt, ch * 128:(ch + 1) * 128], wv[:, kt, :], kt == 0, kt == 7, [wb_, bXT], bb)
                    P.op("act", _mk("activation",
                        out=sz[:, ch, zs * 512:(zs + 1) * 512], in_=bk[:, :], func=AF.Silu), reads=[bb], writes=[bSZ[ch]])
            P.barrier()
            if LIMIT == 1:
                break
            for ch in range(LIMC if LIMIT == 2 else 4):
                c = g * 4 + ch
                ci = c % 16
                cols = slice(ch * 128, (ch + 1) * 128)
                cur, prv = c % 2, (c + 1) % 2
                tS, tm_, el, dtv, av, nac, ea, dd, dte, cd = [sm[:, i, :] for i in range(10)]
                bsm = bf("sm")
                bk, bb = pb()
                for kt in range(8):
                    mm(bk[:, 0:288], xTb[:, kt, cols], wvdt[:, kt, :], kt == 0, kt == 7, [bXT, bf("wvdt")], bb)
                P.op("dve", _mk("tensor_copy", out=Vaug[:, cur, :, 0:64], in_=v3(bk[:, 0:256], 64)), reads=[bb], writes=[bV[cur]])
                P.op("dve", _mk("tensor_tensor", out=tS, in0=bk[:, 256:288], in1=dtb_rep, op=ALU.add), reads=[bb, bf("rep32")], writes=[bsm])
                P.op("dve", _mk("tensor_scalar_min", out=tm_, in0=tS, scalar1=30.0), reads=[bsm], writes=[bsm])
                P.op("act", _mk("activation", out=el, in_=tm_, func=AF.Exp), reads=[bsm], writes=[bsm])
                P.op("act", _mk("activation", out=el, in_=el, func=AF.Ln, bias=1.0), reads=[bsm], writes=[bsm])
                P.op("dve", _mk("tensor_max", out=dtv, in0=tS, in1=el), reads=[bsm], writes=[bsm])
                P.op("dve", _mk("tensor_tensor", out=av, in0=dtv, in1=A_rep[:, :], op=ALU.mult), reads=[bsm, bf("A_rep")], writes=[bsm])
                bk2, bb2 = pb()
                mm(bk2[:, 0:32], tri, av, True, True, [bsm] + CB, bb2)
                mm(bk2[:, 32:64], ones_f[:, :], av, True, True, [bsm] + CB, bb2)
                P.op("dve", _mk("tensor_scalar", out=nac, in0=bk2[:, 0:32], scalar1=-1.0, scalar2=0.0, op0=ALU.mult, op1=ALU.add),
                     reads=[bb2], writes=[bsm])
                P.op("act", _mk("activation", out=ea, in_=bk2[:, 0:32], func=AF.Exp), reads=[bb2], writes=[bsm])
                P.op("dve", _mk("tensor_tensor", out=dd, in0=bk2[:, 32:64], in1=nac, op=ALU.add), reads=[bb2, bsm], writes=[bsm])
                P.op("act", _mk("activation", out=dte, in_=dd, func=AF.Exp), reads=[bsm], writes=[bsm])
                P.op("act", _mk("activation", out=cd, in_=bk2[:, 32:64], func=AF.Exp), reads=[bb2], writes=[bsm])
                bXs, bXtok, bXsw, bBtok = bf("xs"), bf("xtok"), bf("xsw"), bf("Btok")
                for bi in range(2):
                    bk, bb = pb()
                    pbf = bk[:, :].bitcast(BF16)
                    for j in range(8):
                        tr(pbf[:, j * 128:(j + 1) * 128], xbc[:, bi * 8 + j, cols], ident_b, [bXx[ch]] + CB, bb)
                    P.op("act", _mk("activation", func=AF.Copy, out=xtok[:, bi * 1024:(bi + 1) * 1024], in_=pbf), reads=[bb], writes=[bXtok])
                    P.op("dve", _mk("tensor_tensor",
                        out=v3(xs[:, bi * 1024:(bi + 1) * 1024], 64), in0=v3(pbf, 64), in1=bc(dtv[:, bi * 16:(bi + 1) * 16], 64), op=ALU.mult),
                        reads=[bb, bsm, bXtok], writes=[bXs])
                P.op("dve", _mk("tensor_tensor", out=v3(xsw[:, :], 64), in0=v3(xs[:, :], 64), in1=bc(dte, 64), op=ALU.mult),
                     reads=[bXs, bsm], writes=[bXsw])
                bk, bb = pb()
                pbf = bk[:, :].bitcast(BF16)
                for gg in range(4):
                    tr(pbf[:, gg * 128:(gg + 1) * 128], xbc[:, 16 + gg, cols], ident_b, [bXbc[ch]] + CB, bb)
                P.op("act", _mk("activation", func=AF.Copy, out=Btok[:, :, :], in_=v3(pbf[:, 0:512], 128)), reads=[bb], writes=[bBtok])
                bk, bb = pb()
                for gg in range(4):
                    mm(bk[:, gg * 128:(gg + 1) * 128], xbc[:, 16 + gg, cols], xbc[:, 20 + gg, cols], True, True, [bXbc[ch]], bb)
                bcb = bf("cbT")
                P.op("act", _mk("activation", func=AF.Copy, out=cbT[:, :, :], in_=v3(bk[:, :], 128)), reads=[bb], writes=[bcb])
                byz = bf("yz")
                bss = bf("ss")
                P.op("dve", _mk("memset", ss[:, :], 0.0), writes=[bss])
                for gg in range(4):
                    bkA, bbA = pb()
                    for hf in range(2):
                        bkR, bbR = pb()
                        li = (gg * 2 + hf) % 2
                        mi = (gg * 2 + hf) % 4
                        bLT, bMT = bf("LT%d" % li), bf("MT%d" % mi)
                        for j in range(4):
                            h = 8 * gg + 4 * hf + j
                            mm(bkR[:, j * 128:(j + 1) * 128], ident_b, negm[:, 1, 0:128], True, False, CB, bbR)
                            mm(bkR[:, j * 128:(j + 1) * 128], av[:, h:h + 1].to_broadcast([128, 128]), tri, False, True, [bsm] + CB, bbR)
                        for j in range(4):
                            h = 8 * gg + 4 * hf + j
                            P.op("act", _mk("activation",
                                out=LT[li][:, j * 128:(j + 1) * 128], in_=bkR[:, j * 128:(j + 1) * 128], func=AF.Exp, bias=nac[:, h:h + 1]),
                                reads=[bbR, bsm], writes=[bLT])
                        P.op("dve", _mk("tensor_tensor",
                            out=v3(MT[mi][:, :], 128), in0=v3(LT[li][:, :], 128), in1=cbT[:, gg:gg + 1, :].to_broadcast([128, 4, 128]), op=ALU.mult),
                            reads=[bLT, bcb], writes=[bMT])
                        for j in range(4):
                            h = 8 * gg + 4 * hf + j
                            mm(bkA[:, (4 * hf + j) * 64:(4 * hf + j + 1) * 64], MT[mi][:, j * 128:(j + 1) * 128], xs[:, h * 64:(h + 1) * 64],
                               True, True, [bMT, bXs], bbA)
                    gs = slice(gg * 512, (gg + 1) * 512)
                    bkB, bbB = pb()
                    mm(bkB[:, :], xbc[:, 20 + gg, cols], Sbf[:, gs], True, True, [bXbc[ch], bSb[gg]], bbB)
                    bkC, bbC = pb()
                    mm(bkC[:, :], Btok[:, gg, :], xsw[:, gs], True, True, [bBtok, bXsw], bbC)
                    ti_ = gg % 2
                    bt1, bt2 = bf("t1_%d" % ti_), bf("t2_%d" % ti_)
                    P.op("dve", _mk("tensor_tensor",
                        out=v3(t1[ti_][:, :], 64), in0=v3(bkB[:, :], 64), in1=bc(ea[:, 8 * gg:8 * gg + 8], 64), op=ALU.mult),
                        reads=[bbB, bsm], writes=[bt1])
                    P.op("pool", _mk("tensor_tensor",
                        out=v3(t2[ti_][:, :], 64), in0=v3(xtok[:, gs], 64), in1=bc(D_rep[:, 8 * gg:8 * gg + 8], 64), op=ALU.mult),
                        reads=[bXtok, bf("rep32")], writes=[bt2])
                    P.op("pool", _mk("tensor_tensor", out=t1[ti_][:, :], in0=t1[ti_][:, :], in1=t2[ti_][:, :], op=ALU.add),
                         reads=[bt1, bt2], writes=[bt1])
                    P.op("dve", _mk("tensor_tensor", out=yz[:, gs], in0=bkA[:, :], in1=t1[ti_][:, :], op=ALU.add),
                         reads=[bbA, bt1], writes=[byz])
                    P.op("dve", _mk("tensor_tensor", out=yz[:, gs], in0=yz[:, gs], in1=sz[:, ch, gs], op=ALU.mult),
                         reads=[byz, bSZ[ch]], writes=[byz])
                    P.op("act", _mk("activation", out=junk[:, 0:512], in_=yz[:, gs], func=AF.Square, accum_out=ss[:, gg:gg + 1]),
                         reads=[byz, bss], writes=[bss, bf("junk")])
                    P.op("pool", _mk("tensor_tensor",
                        out=v3(S[:, gs], 64), in0=v3(S[:, gs], 64), in1=bc(cd[:, 8 * gg:8 * gg + 8], 64), op=ALU.mult),
                        reads=[bS[gg], bsm], writes=[bS[gg]])
                    P.op("dve", _mk("tensor_tensor", out=S[:, gs], in0=S[:, gs], in1=bkC[:, :], op=ALU.add),
                         reads=[bS[gg], bbC], writes=[bS[gg]])
                    P.op("act", _mk("activation", func=AF.Copy, out=Sbf[:, gs], in_=S[:, gs]), reads=[bS[gg]], writes=[bSb[gg]])
                P.op("dve", _mk("tensor_scalar", out=ss[:, 4:8], in0=ss[:, 0:4], scalar1=1.0 / 512.0, scalar2=1e-5, op0=ALU.mult, op1=ALU.add),
                     reads=[bss], writes=[bss])
                P.op("act", _mk("activation", out=ss[:, 4:8], in_=ss[:, 4:8], func=AF.Ln), reads=[bss], writes=[bss])
                P.op("act", _mk("activation", out=ss[:, 4:8], in_=ss[:, 4:8], func=AF.Exp, scale=-0.5), reads=[bss], writes=[bss])
                byn = bf("yn")
                for gg in range(4):
                    gs = slice(gg * 512, (gg + 1) * 512)
                    P.op("dve", _mk("tensor_scalar", out=yn[:, gs], in0=yz[:, gs], scalar1=ss[:, 4 + gg:5 + gg], scalar2=0.0, op0=ALU.mult, op1=ALU.add),
                         reads=[byz, bss], writes=[byn])
                for bi in range(2):
                    bk, bb = pb()
                    pbf = bk[:, :].bitcast(BF16)
                    for j in range(8):
                        tr(pbf[:, j * 128:(j + 1) * 128], yn[:, (bi * 8 + j) * 128:(bi * 8 + j + 1) * 128], ident_b, [byn] + CB, bb)
                    P.op("dve", _mk("tensor_tensor",
                        out=ynT[:, bi * 8:(bi + 1) * 8, cols], in0=v3(pbf, 128), in1=bc(normw[:, bi * 8:(bi + 1) * 8], 128), op=ALU.mult),
                        reads=[bb, bf("normw")], writes=[bXx[ch]])
                kts = [1] if ci == 0 else [0, 1]
                bat = bf("atok")
                brec = bf("rec")
                for gg in range(4):
                    pi_ = gg % 2
                    bPT = bf("PT%d" % pi_)
                    for kt in kts:
                        kcols = slice((ch + kt) * 128, (ch + kt + 1) * 128)
                        bk, bb = pb()
                        for j in range(4):
                            js = slice(j * 128, (j + 1) * 128)
                            mm(bk[:, js], ident_b, negm[:, kt, 0:128], True, False, CB, bb)
                            mm(bk[:, js], KT[:, gg, kcols], QT[:, j % 2, 2 * gg + j // 2, cols], False, True, [bKT, bQT], bb)
                        P.op("act", _mk("activation", out=PT[pi_][:, kt, :], in_=bk[:, :], func=AF.Exp, scale=0.125),
                             reads=[bb], writes=[bPT])
                    bkO, bbO = pb()
                    for jj in range(4):
                        j = jj
                        for kt in kts:
                            blk = cur if kt == 1 else prv
                            mm(bkO[:, j * 65:(j + 1) * 65], PT[pi_][:, kt, jj * 128:(jj + 1) * 128], Vaug[:, blk, gg, 0:65],
                               kt == kts[0], kt == kts[-1], [bPT, bV[blk]], bbO)
                    O3 = v3(bkO[:, 0:260], 65)
                    P.op("dve", _mk("tensor_tensor", out=rec[:, 0:4], in0=O3[:, :, 64], in1=esink[:, 4 * gg:4 * gg + 4], op=ALU.add),
                         reads=[bbO, bf("esink")], writes=[brec])
                    P.op("dve", _mk("reciprocal", out=rec[:, 4:8], in_=rec[:, 0:4]), reads=[brec], writes=[brec])
                    P.op("dve", _mk("tensor_tensor",
                        out=v3(atok[:, gg * 256:(gg + 1) * 256], 64), in0=O3[:, :, 0:64], in1=bc(rec[:, 4:8], 64), op=ALU.mult),
                        reads=[bbO, brec], writes=[bat])
                bk, bb = pb()
                pbf = bk[:, :].bitcast(BF16)
                for j in range(8):
                    tr(pbf[:, j * 128:(j + 1) * 128], atok[:, j * 128:(j + 1) * 128], ident_b, [bat] + CB, bb)
                P.op("act", _mk("activation", func=AF.Copy, out=aT[:, :, cols], in_=v3(pbf, 128)), reads=[bb], writes=[bXbc[ch]])
            P.barrier()
            if LIMIT == 2:
                break
            bMg = bf("mergedT")
            for pi in range(4):
                ri = pi % 2
                bG1, bG0 = bf("g1m%d" % ri), bf("g0t%d" % ri)
                wv, wb_ = slab_load(wgp_v[:, :, pi * 512:(pi + 1) * 512], (8, 512))
                gb = []
                for q4 in range(4):
                    bk, bb = pb()
                    for kt in range(8):
                        mm(bk[:, :], wv[:, kt, q4 * 128:(q4 + 1) * 128], xTb[:, kt, :], kt == 0, kt == 7, [wb_, bXT], bb)
                    i2 = q4 % 2
                    if q4 < 2:
                        P.op("act", _mk("activation",
                            out=g0t[ri][:, i2, :], in_=bk[:, :], func=AF.Sigmoid, bias=bgate[:, 2 * pi + i2:2 * pi + i2 + 1]),
                            reads=[bb, bf("bgate")], writes=[bG0])
                    else:
                        P.op("act", _mk("activation",
                            out=g1m[ri][:, i2, :], in_=bk[:, :], func=AF.Sigmoid, bias=bgate[:, 8 + 2 * pi + i2:8 + 2 * pi + i2 + 1]),
                            reads=[bb, bf("bgate")], writes=[bG1])
                wv, wb_ = slab_load(wao_v[:, :, pi * 256:(pi + 1) * 256], (8, 256))
                for i2 in range(2):
                    bk, bb = pb()
                    for kt in range(8):
                        mm(bk[:, :], wv[:, kt, i2 * 128:(i2 + 1) * 128], aT[:, kt, :], kt == 0, kt == 7, [wb_] + bXbc, bb)
                    P.op("dve", _mk("tensor_tensor", out=g1m[ri][:, i2, :], in0=bk[:, :], in1=g1m[ri][:, i2, :], op=ALU.mult),
                         reads=[bb, bG1], writes=[bG1])
                wv, wb_ = slab_load(wso_v[:, :, pi * 256:(pi + 1) * 256], (16, 256))
                for i2 in range(2):
                    bk, bb = pb()
                    for kt in range(16):
                        mm(bk[:, :], wv[:, kt, i2 * 128:(i2 + 1) * 128], ynT[:, kt, :], kt == 0, kt == 15, [wb_] + bXx, bb)
                    P.op("dve", _mk("tensor_tensor", out=g0t[ri][:, i2, :], in0=bk[:, :], in1=g0t[ri][:, i2, :], op=ALU.mult),
                         reads=[bb, bG0], writes=[bG0])
                    P.op("pool", _mk("tensor_tensor",
                        out=mergedT[:, 2 * pi + i2, :], in0=g0t[ri][:, i2, :], in1=g1m[ri][:, i2, :], op=ALU.add),
                        reads=[bG0, bG1], writes=[bMg])
            if LIMIT == 3:
                break
            wmx = []
            for hf in range(2):
                wmx.append(slab_load(wmix_v[:, :, hf * 512:(hf + 1) * 512], (8, 512)))
            bXt, bH, bHbf, bHT, bSt = bf("xt32"), bf("hh"), bf("hbf"), bf("hT"), bf("st4")
            bLg, bOh, bMm, bMb, bTt, bDf, bEx = bf("lg"), bf("oh"), bf("Mm"), bf("Mb"), bf("Tt"), bf("destf"), bf("exv")
            for ch in range(4):
                c = g * 4 + ch
                cols = slice(ch * 128, (ch + 1) * 128)
                rows = slice(c * 128, (c + 1) * 128)
                P.op("sp", _mk("dma_start", out=xt32[:, :], in_=x_tok[rows, :]), writes=[bXt], dma="xt32")
                for hf in range(2):
                    wv, wb_ = wmx[hf]
                    bk, bb = pb()
                    for kt in range(8):
                        mm(bk[:, :], mergedT[:, kt, cols], wv[:, kt, :], kt == 0, kt == 7, [bMg, wb_], bb)
                    hs = slice(hf * 512, (hf + 1) * 512)
                    P.op("dve", _mk("scalar_tensor_tensor",
                        out=hh[:, hs], in0=xt32[:, hs], scalar=ALPHA, in1=bk[:, :], op0=ALU.mult, op1=ALU.add),
                        reads=[bXt, bb], writes=[bH])
                emit_ln(P, hh, st4, hT, bH, bSt, bHT, lnrep[:, 0, :], lnrep[:, 1, :], bf("lnrep"))
                P.op("sp", _mk("dma_start", out=hbuf[rows, :], in_=hh[:, :]), reads=[bH], writes=[bHB], dma="hst", nowaw=True)
                P.op("act", _mk("activation", func=AF.Copy, out=hbf[:, :], in_=hh[:, :]), reads=[bH], writes=[bHbf])
                for b2 in range(2):
                    bk, bb = pb()
                    for j in range(4):
                        jj = b2 * 4 + j
                        tr(bk[:, j * 128:(j + 1) * 128], hh[:, jj * 128:(jj + 1) * 128], ident_f, [bH] + CB, bb)
                    P.op("act", _mk("activation", func=AF.Copy, out=hT[:, b2 * 4:(b2 + 1) * 4, :], in_=v3(bk[:, :], 128)), reads=[bb], writes=[bHT])
                bk, bb = pb()
                for kt in range(8):
                    mm(bk[:, 0:32], hT[:, kt, :], wrt[:, kt, :], kt == 0, kt == 7, [bHT, bf("wrt")], bb)
                P.op("dve", _mk("tensor_tensor", out=lg[:, :], in0=bk[:, 0:32], in1=brt_rep, op=ALU.add), reads=[bb, bf("rep32")], writes=[bLg])
                for k in range(4):
                    P.op("dve", _mk("reduce_max", out=st4[:, 8 + k:9 + k], in_=lg[:, :], axis=AX.X), reads=[bLg], writes=[bSt])
                    P.op("dve", _mk("tensor_scalar", out=oh[:, k, :], in0=lg[:, :], scalar1=st4[:, 8 + k:9 + k], scalar2=0.0,
                                                               op0=ALU.is_equal, op1=ALU.add), reads=[bLg, bSt], writes=[bOh])
                    P.op("dve", _mk("scalar_tensor_tensor", out=lg[:, :], in0=oh[:, k, :], scalar=-1e9, in1=lg[:, :], op0=ALU.mult, op1=ALU.add),
                         reads=[bOh, bLg], writes=[bLg])
                P.op("dve", _mk("tensor_tensor", out=Mm[:, :], in0=oh[:, 0, :], in1=oh[:, 1, :], op=ALU.add), reads=[bOh], writes=[bMm])
                P.op("dve", _mk("tensor_tensor", out=Mm[:, :], in0=Mm[:, :], in1=oh[:, 2, :], op=ALU.add), reads=[bOh, bMm], writes=[bMm])
                P.op("dve", _mk("tensor_tensor", out=Mb[:, :], in0=Mm[:, :], in1=oh[:, 3, :], op=ALU.add), reads=[bOh, bMm], writes=[bMb])
                P.op("dve", _mk("tensor_scalar", out=st4[:, 13:14], in0=st4[:, 8:9], scalar1=-1.0, scalar2=0.0, op0=ALU.mult, op1=ALU.add),
                     reads=[bSt], writes=[bSt])
                P.op("dve", _mk("memset", st4[:, 12:13], 0.0), reads=[bSt], writes=[bSt])
                P.op("act", _mk("activation", out=exv[:, :], in_=st4[:, 8:12], func=AF.Exp, bias=st4[:, 13:14], accum_out=st4[:, 12:13]),
                     reads=[bSt], writes=[bSt, bEx])
                P.op("dve", _mk("reciprocal", out=st4[:, 14:15], in_=st4[:, 12:13]), reads=[bSt], writes=[bSt])
                P.op("dve", _mk("tensor_scalar", out=gatew[:, c, :], in0=exv[:, :], scalar1=st4[:, 14:15], scalar2=0.0, op0=ALU.mult, op1=ALU.add), reads=[bSt, bEx], writes=[bGw])
                bk, bb = pb()
                mm(bk[:, 0:32], stri_b, Mb[:, :], True, True, [bMb] + CB, bb)
                mm(bk[:, 32:64], ones_b[:, :], Mb[:, :], True, True, [bMb] + CB, bb)
                P.op("dve", _mk("tensor_tensor", out=Tt[:, 0, :], in0=bk[:, 0:32], in1=cnt[:, :], op=ALU.add), reads=[bb, bf("cnt")], writes=[bTt])
                P.op("dve", _mk("tensor_scalar", out=Tt[:, 1, :], in0=Tt[:, 0, :], scalar1=CAP - 0.5, scalar2=1.0, op0=ALU.is_ge, op1=ALU.mult),
                     reads=[bTt], writes=[bTt])
                P.op("dve", _mk("tensor_tensor", out=Tt[:, 0, :], in0=Tt[:, 0, :], in1=ebase, op=ALU.add), reads=[bTt, bf("rep32")], writes=[bTt])
                P.op("dve", _mk("tensor_tensor", out=Tt[:, 2, :], in0=dumpT, in1=Tt[:, 0, :], op=ALU.subtract), reads=[bTt, bf("rep32")], writes=[bTt])
                P.op("dve", _mk("tensor_tensor", out=Tt[:, 2, :], in0=Tt[:, 2, :], in1=Tt[:, 1, :], op=ALU.mult), reads=[bTt], writes=[bTt])
                P.op("dve", _mk("tensor_tensor", out=Tt[:, 0, :], in0=Tt[:, 0, :], in1=Tt[:, 2, :], op=ALU.add), reads=[bTt], writes=[bTt])
                P.op("dve", _mk("tensor_tensor", out=cnt[:, :], in0=cnt[:, :], in1=bk[:, 32:64], op=ALU.add), reads=[bb, bf("cnt")], writes=[bf("cnt")])
                for k in range(4):
                    P.op("dve", _mk("tensor_tensor", out=Tt[:, 2, :], in0=oh[:, k, :], in1=Tt[:, 0, :], op=ALU.mult), reads=[bOh, bTt], writes=[bTt])
                    P.op("dve", _mk("reduce_sum", out=destf[:, k:k + 1], in_=Tt[:, 2, :], axis=AX.X), reads=[bTt], writes=[bDf])
                P.op("dve", _mk("tensor_copy", out=dest[:, c, :], in_=destf[:, :]), reads=[bDf], writes=[bDest])
                for k in range(0 if NO_SCATTER else 4):
                    P.op("pool", _mk("indirect_dma_start",
                        out=Xg[:, :], out_offset=bass.IndirectOffsetOnAxis(ap=dest2[:, c * 4 + k:c * 4 + k + 1], axis=0), in_=hbf[:, :], in_offset=None), reads=[bHbf, bDest], writes=[bXG], dma="sc", nowaw=not first_sc[0])
                    first_sc[0] = False
            P.barrier(extra=[bXt, bH, bHbf])

        aoff[0] = 0
        NE_ = 0 if STOP == 1 else NEXP
        NC3 = 0 if STOP in (1, 2) else NGROUPS * 4
        wgu = [alloc([128, 8, 2048], BF16) for _ in range(2)]
        wdn = [alloc([128, 8, 1024], BF16) for _ in range(2)]
        bdr = [alloc([128, 1024], F32) for _ in range(2)]
        xgt = [alloc([128, 1024], BF16) for _ in range(6)]
        XgT = alloc([128, 8, CAP], BF16)
        actT = alloc([128, 8, CAP], BF16)
        glu = [alloc([128, 384], F32) for _ in range(2)]
        sig = [alloc([128, 384], F32) for _ in range(2)]
        lin = [alloc([128, 384], F32) for _ in range(2)]
        Yt = [alloc([128, 1024], F32) for _ in range(2)]
        p2_end = aoff[0]
        bWgu, bWdn, bBdr = [bf("wgu0"), bf("wgu1")], [bf("wdn0"), bf("wdn1")], [bf("bdr0"), bf("bdr1")]
        bXgt = [bf("xgt%d" % i_) for i_ in range(6)]
        bXgT, bActT = bf("XgT"), bf("actT")
        bGlu, bSig, bLin = [bf("glu0"), bf("glu1")], [bf("sig0"), bf("sig1")], [bf("lin0"), bf("lin1")]
        bYt = [bf("Yt0"), bf("Yt1")]

        def expert_loads(e_):
            s = e_ % 2
            for q in range(4):
                P.op("pool", _mk("dma_start",
                    out=wgu[s][:, :, q * 512:(q + 1) * 512], in_=w_gu[e_].rearrange("(kt p) c -> p kt c", p=128)[:, :, q * 512:(q + 1) * 512]),
                    writes=[bWgu[s]], dma="wgu%d" % s, nowaw=(q > 0))
            for q in range(2):
                P.op("pool", _mk("dma_start",
                    out=wdn[s][:, :, q * 512:(q + 1) * 512], in_=w_dn[e_].rearrange("(kt p) c -> p kt c", p=128)[:, :, q * 512:(q + 1) * 512]),
                    writes=[bWdn[s]], dma="wdn%d" % s, nowaw=(q > 0))
            P.op("sp", _mk("dma_start", out=bdr[s][:, :], in_=bd_rep[e_]), writes=[bBdr[s]], dma="bdr%d" % s)

        def xgt_loads(e_):
            for stl in range(6):
                r0 = e_ * CAP + stl * 128
                P.op("sp", _mk("dma_start", out=xgt[stl][:, :], in_=Xg[r0:r0 + 128, :]), reads=[bXG], writes=[bXgt[stl]], dma="xgt%d" % stl)

        if NE_:
            expert_loads(0)
            xgt_loads(0)
        for e_ in range(NE_):
            s = e_ % 2
            if e_ + 1 < NE_:
                expert_loads(e_ + 1)
            for stl in range(6):
                bk, bb = pb()
                pbf = bk[:, :].bitcast(BF16)
                for j in range(8):
                    tr(pbf[:, j * 128:(j + 1) * 128], xgt[stl][:, j * 128:(j + 1) * 128], ident_b, [bXgt[stl]] + CB, bb)
                P.op("act", _mk("activation", func=AF.Copy, out=XgT[:, :, stl * 128:(stl + 1) * 128], in_=v3(pbf, 128)), reads=[bb], writes=[bXgT])
            if e_ + 1 < NE_:
                xgt_loads(e_ + 1)
            for fi in range(8):
                for hf in range(2):
                    cs = slice(hf * 384, (hf + 1) * 384)
                    r = (fi * 2 + hf) % 2
                    bkG, bbG = pb()
                    for kt in range(8):
                        mm(bkG[:, 0:384], wgu[s][:, kt, fi * 128:(fi + 1) * 128], XgT[:, kt, cs], kt == 0, kt == 7, [bWgu[s], bXgT], bbG)
                    bkL, bbL = pb()
                    for kt in range(8):
                        mm(bkL[:, 0:384], wgu[s][:, kt, 1024 + fi * 128:1024 + (fi + 1) * 128], XgT[:, kt, cs], kt == 0, kt == 7, [bWgu[s], bXgT], bbL)
                    P.op("dve", _mk("tensor_scalar",
                        out=glu[r][:, :], in0=bkG[:, 0:384], scalar1=bgu[:, e_, fi:fi + 1], scalar2=7.0, op0=ALU.add, op1=ALU.min),
                        reads=[bbG, bf("bgu")], writes=[bGlu[r]])
                    P.op("act", _mk("activation", out=sig[r][:, :], in_=glu[r][:, :], func=AF.Sigmoid, scale=1.702), reads=[bGlu[r]], writes=[bSig[r]])
                    P.op("dve", _mk("tensor_scalar",
                        out=lin[r][:, :], in0=bkL[:, 0:384], scalar1=bgu[:, e_, 8 + fi:9 + fi], scalar2=7.0, op0=ALU.add, op1=ALU.min),
                        reads=[bbL, bf("bgu")], writes=[bLin[r]])
                    P.op("dve", _mk("tensor_scalar", out=lin[r][:, :], in0=lin[r][:, :], scalar1=-7.0, scalar2=1.0, op0=ALU.max, op1=ALU.add),
                         reads=[bLin[r]], writes=[bLin[r]])
                    P.op("pool", _mk("tensor_tensor", out=sig[r][:, :], in0=sig[r][:, :], in1=glu[r][:, :], op=ALU.mult),
                         reads=[bSig[r], bGlu[r]], writes=[bSig[r]])
                    P.op("pool", _mk("tensor_tensor", out=actT[:, fi, cs], in0=sig[r][:, :], in1=lin[r][:, :], op=ALU.mult),
                         reads=[bSig[r], bLin[r]], writes=[bActT])
            for stl in range(6):
                ys = stl % 2
                r0 = e_ * CAP + stl * 128
                for hf in range(2):
                    hs = slice(hf * 512, (hf + 1) * 512)
                    bk, bb = pb()
                    for ft in range(8):
                        mm(bk[:, :], actT[:, ft, stl * 128:(stl + 1) * 128], wdn[s][:, ft, hs], ft == 0, ft == 7, [bActT, bWdn[s]], bb)
                    P.op("dve", _mk("tensor_tensor", out=Yt[ys][:, hs], in0=bk[:, :], in1=bdr[s][:, hs], op=ALU.add),
                         reads=[bb, bBdr[s]], writes=[bYt[ys]])
                P.op("sp", _mk("dma_start", out=Yg[r0:r0 + 128, :], in_=Yt[ys][:, :]), reads=[bYt[ys]], writes=[bYG], dma="yst%d" % ys, nowaw=True)
        P.barrier(extra=bYt + bXgt)

        aoff[0] = 0
        yk = [[alloc([128, 1024], F32) for _ in range(4)] for _ in range(2)]
        h3 = [alloc([128, 1024], F32) for _ in range(2)]
        ac3 = [alloc([128, 1024], F32) for _ in range(2)]
        jk3 = alloc([128, 1024], F32)
        st3 = alloc([128, 16], F32)
        bYk = [[bf("yk%d_%d" % (r, k)) for k in range(4)] for r in range(2)]
        bH3, bAc3 = [bf("h3_0"), bf("h3_1")], [bf("ac3_0"), bf("ac3_1")]
        bJk3, bSt3 = bf("jk3"), bf("st3")
        for c in range(NC3):
            r = c % 2
            rows = slice(c * 128, (c + 1) * 128)
            for k in range(4):
                P.op("pool", _mk("indirect_dma_start",
                    out=yk[r][k][:, :], out_offset=None, in_=Yg[:, :], in_offset=bass.IndirectOffsetOnAxis(ap=dest2[:, c * 4 + k:c * 4 + k + 1], axis=0)), reads=[bYG, bDest], writes=[bYk[r][k]], dma="yk%d_%d" % (r, k))
            P.op("sp", _mk("dma_start", out=h3[r][:, :], in_=hbuf[rows, :]), reads=[bHB], writes=[bH3[r]], dma="h3_%d" % r)
            a3 = ac3[r]
            P.op("dve", _mk("tensor_scalar", out=a3[:, :], in0=yk[r][0][:, :], scalar1=gatew[:, c, 0:1], scalar2=0.0, op0=ALU.mult, op1=ALU.add),
                 reads=[bYk[r][0], bGw], writes=[bAc3[r]])
            for k in range(1, 4):
                P.op("dve", _mk("scalar_tensor_tensor",
                    out=a3[:, :], in0=yk[r][k][:, :], scalar=gatew[:, c, k:k + 1], in1=a3[:, :], op0=ALU.mult, op1=ALU.add),
                    reads=[bYk[r][k], bGw, bAc3[r]], writes=[bAc3[r]])
            P.op("dve", _mk("scalar_tensor_tensor", out=a3[:, :], in0=h3[r][:, :], scalar=ALPHA, in1=a3[:, :], op0=ALU.mult, op1=ALU.add),
                 reads=[bH3[r], bAc3[r]], writes=[bAc3[r]])
            emit_ln(P, a3, st3, jk3, bAc3[r], bSt3, bJk3, lnrep[:, 2, :], lnrep[:, 3, :], bf("lnrep"))
            P.op("sp", _mk("dma_start", out=out_d[rows, :], in_=a3[:, :]), reads=[bAc3[r]], writes=[bOUT], dma="out%d" % r, nowaw=True)
        P.op("sp", _mk("nop", ), reads=[bOUT, bHB, bXT] + sl_b, force=True)
        P.run()
    return nc


def emit_ln(P, x, st, junk, bX, bSt, bJ, g_rep, b_rep, bLn):
    jv = junk if len(junk.shape) == 2 else junk.rearrange("p a b -> p (a b)")
    P.op("dve", _mk("memset", st[:, 0:4], 0.0), reads=[bSt], writes=[bSt])
    P.op("act", _mk("activation", out=jv[:, 0:1024], in_=x[:, :], func=AF.Identity, accum_out=st[:, 0:1]), reads=[bX, bSt], writes=[bSt, bJ])
    P.op("dve", _mk("tensor_scalar", out=st[:, 1:2], in0=st[:, 0:1], scalar1=-1.0 / 1024.0, scalar2=0.0, op0=ALU.mult, op1=ALU.add),
         reads=[bSt], writes=[bSt])
    P.op("act", _mk("activation", out=jv[:, 0:1024], in_=x[:, :], func=AF.Square, bias=st[:, 1:2], accum_out=st[:, 2:3]),
         reads=[bX, bSt], writes=[bSt, bJ])
    P.op("act", _mk("activation", out=st[:, 3:4], in_=st[:, 2:3], func=AF.Ln, scale=1.0 / 1024.0, bias=1e-5), reads=[bSt], writes=[bSt])
    P.op("act", _mk("activation", out=st[:, 4:5], in_=st[:, 3:4], func=AF.Exp, scale=-0.5), reads=[bSt], writes=[bSt])
    P.op("dve", _mk("tensor_scalar", out=x[:, :], in0=x[:, :], scalar1=st[:, 1:2], scalar2=st[:, 4:5], op0=ALU.add, op1=ALU.mult),
         reads=[bX, bSt], writes=[bX])
    P.op("pool", _mk("tensor_tensor", out=x[:, :], in0=x[:, :], in1=g_rep, op=ALU.mult), reads=[bX, bLn], writes=[bX])
    P.op("pool", _mk("tensor_tensor", out=x[:, :], in0=x[:, :], in1=b_rep, op=ALU.add), reads=[bX, bLn], writes=[bX])


def _consts():
    k = np.arange(128)[:, None]
    t = np.arange(128)[None, :]
    ident = (k == t).astype(np.float32)
    tri = (k <= t).astype(np.float32)
    stri = (k < t).astype(np.float32)
    negm_cur = np.where(k <= t, 0.0, NEG).astype(np.float32)
    negm_prev = np.where(k > t, 0.0, NEG).astype(np.float32)
    return np.concatenate([ident, tri, stri, negm_cur, negm_prev], axis=1)


def _prep(inp):
    f = lambda a: np.ascontiguousarray(np.asarray(a, dtype=np.float32))
    w_in = f(inp["w_in"])[0]
    o = np.cumsum((0, 2048, 3072, 32, 1024, 256, 256, 2048))
    z, xbc, dt, q, k, v, gts = [w_in[:, o[i]:o[i + 1]] for i in range(7)]
    kdup = np.concatenate([np.concatenate([k[:, g * 64:(g + 1) * 64]] * 2, axis=1) for g in range(4)], axis=1)
    w_fm = np.concatenate([xbc, q, kdup], axis=1)
    w_vdt = np.concatenate([v, dt], axis=1)
    g0, g1 = gts[:, :1024], gts[:, 1024:]
    w_gp = np.concatenate([np.concatenate([g0[:, p * 256:(p + 1) * 256], g1[:, p * 256:(p + 1) * 256]], axis=1) for p in range(4)], axis=1)
    pl = lambda vec, nt: f(vec).reshape(nt, 128).T
    conv_w = f(inp["conv_w"])[0]
    convw_l = np.stack([pl(conv_w[j], 24) for j in range(4)], axis=2).reshape(128, 96)
    rep = lambda vec: np.broadcast_to(f(vec).reshape(1, -1), (128, f(vec).size))
    sinks = np.zeros(32, np.float32)
    sinks[:16] = f(inp["attn_sinks"])[0]
    ebase = (np.arange(32) * CAP).astype(np.float32)
    rep32 = np.concatenate([rep(inp["dt_bias"][0]), rep(inp["a_log"][0]), rep(inp["d_skip"][0]), rep(inp["b_router"][0]), rep(sinks), rep(ebase), np.broadcast_to((NEXP * CAP + np.arange(128, dtype=np.float32))[:, None], (128, 32))], axis=1)
    ln_rep = np.concatenate([rep(inp["ln1_g"][0]), rep(inp["ln1_b"][0]), rep(inp["ln2_g"][0]), rep(inp["ln2_b"][0])], axis=1)
    bgu = f(inp["b_gate_up"])[0]
    bgu_l = np.stack([pl(bgu[e], 16) for e in range(NEXP)], axis=1).reshape(128, NEXP * 16)
    bd = f(inp["b_down"])[0]
    shared = dict(
        w_fm=f(w_fm), w_z=f(z), w_vdt=f(w_vdt), w_gp=f(w_gp), w_so=f(inp["w_ssm_out"])[0], w_ao=f(inp["w_attn_out"])[0],
        w_mix=f(inp["w_mix_out"])[0], w_rt=f(inp["w_router"])[0], w_gu=f(inp["w_gate_up"])[0], w_dn=f(inp["w_down"])[0],
        bd_rep=f(np.broadcast_to(bd[:, None, :], (NEXP, 128, 1024))),
        convw_l=f(convw_l), convb_l=f(pl(inp["conv_b"][0], 24)), bgate_l=f(pl(inp["b_gates"][0], 16)),
        normw_l=f(pl(inp["ssm_norm_w"][0], 16)), bgu_l=f(bgu_l), rep32=f(rep32), ln_rep=f(ln_rep), cst=f(_consts()))
    x = f(inp["x"])
    maps = []
    for c in range(NCORES):
        xt = x[2 * c:2 * c + 2].reshape(NT, 1024)
        m = dict(shared)
        m["x_tok"] = np.ascontiguousarray(xt)
        m["xT"] = np.ascontiguousarray(xt.T)
        maps.append(m)
    return maps


def kernel(**inputs):
    maps = _prep(inputs)
    nc = build_nc()
    res = run_bass_kernel_spmd(nc, maps, core_ids=list(range(NCORES)))
    out = np.concatenate([np.asarray(r["out"]).reshape(2, 2048, 1024) for r in res.results], axis=0)
    return out.astype(np.float32)
```
